# Optimizing a Trainium2 kernel written in Bass

```python
import math
import jax, jax.numpy as jnp
from jax import lax
import numpy as np


D_MODEL = 1024
BATCH = 16
SEQ = 2048
DEPTH = 1

MEM_LEN = 256
DA_HEADS = 8
DA_DK = D_MODEL // (2 * DA_HEADS)
DA_DV = 2 * DA_DK
Q_BLOCK = 128
RET_HEADS = 4
RET_DV = D_MODEL // RET_HEADS
RET_DK = RET_DV // 2
RET_CHUNK = 128
MEM_HEADS = 4
MEM_DH = D_MODEL // MEM_HEADS
N_BRANCH = 3
BRANCH_W = D_MODEL
N_GROUPS = 4
EXPERTS_PER_GROUP = 4
N_EXPERTS = N_GROUPS * EXPERTS_PER_GROUP
EXPERT_TOPK = 2
D_FF_EXPERT = D_MODEL // 2
DEEPNORM_ALPHA = (2.0 * DEPTH) ** 0.25
DEEPNORM_BETA = (8.0 * DEPTH) ** -0.25
EPS = 1e-5

SPLIT_SIZES = (DA_HEADS * 2 * DA_DK, DA_HEADS * 2 * DA_DK, DA_HEADS * DA_DV,
               RET_HEADS * RET_DK, RET_HEADS * RET_DK, RET_HEADS * RET_DV, RET_HEADS * RET_DV,
               MEM_HEADS * MEM_DH, N_BRANCH * D_MODEL)
SPLIT_POINTS = tuple(int(v) for v in np.cumsum(SPLIT_SIZES)[:-1])
IN_WIDTH = int(sum(SPLIT_SIZES))
VALUE_SLOTS = (2, 5)

kernel_name = 'hybrid_diffattn_retention_memxattn_hmoe_deepnorm'


def layer_norm(x, g, b):
    xf = x.astype(jnp.float32)
    mu = jnp.mean(xf, axis=-1, keepdims=True)
    var = jnp.mean(jnp.square(xf - mu), axis=-1, keepdims=True)
    return ((xf - mu) * lax.rsqrt(var + EPS) * g + b).astype(x.dtype)


def diff_attention(q, k, v, lam_vecs, norm_g, lambda_init):
    B, S, H = q.shape[0], q.shape[1], q.shape[2]
    lv = lam_vecs.astype(jnp.float32)
    lam = jnp.exp(jnp.sum(lv[0] * lv[1])) - jnp.exp(jnp.sum(lv[2] * lv[3])) + lambda_init
    slopes = 2.0 ** (-8.0 * (jnp.arange(H, dtype=jnp.float32) + 1.0) / H)
    kf = k.astype(jnp.float32)
    vf = v.astype(jnp.float32)
    kpos = jnp.arange(S)
    nblk = S // Q_BLOCK
    qb = q.astype(jnp.float32).reshape(B, nblk, Q_BLOCK, H, 2, DA_DK).transpose(1, 0, 2, 3, 4, 5)
    scale = DA_DK ** -0.5

    def block(args):
        q_blk, i = args
        qpos = i * Q_BLOCK + jnp.arange(Q_BLOCK)
        dist = (qpos[:, None] - kpos[None, :]).astype(jnp.float32)
        s = jnp.einsum('bqhcd,bkhcd->bhcqk', q_blk, kf) * scale
        s = s - slopes[None, :, None, None, None] * dist[None, None, None]
        s = jnp.where(dist[None, None, None] >= 0, s, -jnp.inf)
        p = jax.nn.softmax(s, axis=-1)
        a = p[:, :, 0] - lam * p[:, :, 1]
        return jnp.einsum('bhqk,bkhe->bqhe', a, vf)

    o = lax.map(block, (qb, jnp.arange(nblk)))
    o = o.transpose(1, 0, 2, 3, 4).reshape(B, S, H, DA_DV)
    o = o * lax.rsqrt(jnp.mean(jnp.square(o), axis=-1, keepdims=True) + EPS) * norm_g
    return (o * (1.0 - lambda_init)).reshape(B, S, H * DA_DV)


def retention(q, k, v, gn_g, gn_b):
    B, S, H, dk = q.shape
    dv = v.shape[-1]
    C = RET_CHUNK
    n = S // C
    log_g = jnp.log(1.0 - 2.0 ** (-5.0 - jnp.arange(H, dtype=jnp.float32)))
    idx = jnp.arange(C, dtype=jnp.float32)
    rel = idx[:, None] - idx[None, :]
    intra = jnp.where(rel[None] >= 0, jnp.exp(rel[None] * log_g[:, None, None]), 0.0)
    q_decay = jnp.exp((idx[:, None] + 1.0) * log_g[None, :])
    k_decay = jnp.exp((C - 1.0 - idx[:, None]) * log_g[None, :])
    chunk_decay = jnp.exp(C * log_g)
    qf = q.astype(jnp.float32)
    kf = k.astype(jnp.float32) * dk ** -0.5
    vf = v.astype(jnp.float32)
    qc = qf.reshape(B, n, C, H, dk).transpose(1, 0, 2, 3, 4)
    kc = kf.reshape(B, n, C, H, dk).transpose(1, 0, 2, 3, 4)
    vc = vf.reshape(B, n, C, H, dv).transpose(1, 0, 2, 3, 4)

    def step(state, inp):
        qi, ki, vi = inp
        s = jnp.einsum('bqhd,bkhd->bhqk', qi, ki) * intra[None]
        o = (jnp.einsum('bhqk,bkhe->bqhe', s, vi)
             + jnp.einsum('bqhd,bhde->bqhe', qi, state) * q_decay[None, :, :, None])
        new_state = (state * chunk_decay[None, :, None, None]
                     + jnp.einsum('bkhd,bkhe->bhde', ki * k_decay[None, :, :, None], vi))
        return new_state, o

    state0 = jnp.zeros((B, H, dk, dv), jnp.float32)
    _, o = lax.scan(step, state0, (qc, kc, vc))
    o = o.transpose(1, 0, 2, 3, 4).reshape(B, S, H, dv)
    mu = jnp.mean(o, axis=-1, keepdims=True)
    var = jnp.mean(jnp.square(o - mu), axis=-1, keepdims=True)
    o = ((o - mu) * lax.rsqrt(var + EPS)).reshape(B, S, H * dv)
    return o * gn_g + gn_b


def memory_attention(q, k, v):
    B, S = q.shape[0], q.shape[1]
    s = jnp.einsum('bshd,bmhd->bhsm', q.astype(jnp.float32), k.astype(jnp.float32)) * MEM_DH ** -0.5
    p = jax.nn.softmax(s, axis=-1)
    o = jnp.einsum('bhsm,bmhd->bshd', p, v.astype(jnp.float32))
    return o.reshape(B, S, MEM_HEADS * MEM_DH)


def hier_moe(x, w_rg, b_rg, w_re, b_re, w1, w3, w2):
    B, S, D = x.shape
    t = x.reshape(B * S, D)
    p_grp = jax.nn.softmax((t @ w_rg + b_rg).astype(jnp.float32), axis=-1)
    p_top, g_idx = lax.top_k(p_grp, 1)
    e_logits = (t @ w_re + b_re).astype(jnp.float32).reshape(B * S, N_GROUPS, EXPERTS_PER_GROUP)
    sel = jnp.take_along_axis(e_logits, g_idx[:, :, None], axis=1)[:, 0]
    p_exp = jax.nn.softmax(sel, axis=-1)
    pe_top, e_idx = lax.top_k(p_exp, EXPERT_TOPK)
    pe_top = pe_top / jnp.sum(pe_top, axis=-1, keepdims=True)
    w_tok = p_top * pe_top
    flat_idx = g_idx * EXPERTS_PER_GROUP + e_idx
    combine = jnp.sum(jax.nn.one_hot(flat_idx, N_EXPERTS, dtype=jnp.float32) * w_tok[..., None], axis=1)
    out = jnp.zeros((B * S, D), jnp.float32)
    for e in range(N_EXPERTS):
        h = jax.nn.silu(t @ w1[e]) * (t @ w3[e])
        out = out + combine[:, e:e + 1] * (h @ w2[e])
    return out.reshape(B, S, D).astype(x.dtype)


def setup_inputs(seed: int = 0) -> dict:
    key = jax.random.key(seed)
    ks = jax.random.split(key, 24)
    D = D_MODEL

    def nrm(k, shape, scale):
        return jax.random.normal(k, shape, jnp.float32) * scale

    col_scale = np.concatenate([np.full(sz, DEEPNORM_BETA if i in VALUE_SLOTS else 1.0, np.float32)
                                for i, sz in enumerate(SPLIT_SIZES)])
    mem_scale = np.concatenate([np.ones(D, np.float32), np.full(D, DEEPNORM_BETA, np.float32)])
    return {
        'x': nrm(ks[0], (BATCH, SEQ, D), 1.0),
        'mem': nrm(ks[1], (BATCH, MEM_LEN, D), 1.0),
        'w_in': nrm(ks[2], (DEPTH, D, IN_WIDTH), D ** -0.5) * jnp.asarray(col_scale),
        'b_gate': nrm(ks[3], (DEPTH, N_BRANCH * D), 0.02),
        'w_mem_kv': nrm(ks[4], (DEPTH, D, 2 * D), D ** -0.5) * jnp.asarray(mem_scale),
        'da_lambda': nrm(ks[5], (DEPTH, 4, DA_DK), 0.1),
        'da_norm_g': 1.0 + nrm(ks[6], (DEPTH, DA_DV), 0.02),
        'ret_gn_g': 1.0 + nrm(ks[7], (DEPTH, RET_HEADS * RET_DV), 0.02),
        'ret_gn_b': nrm(ks[8], (DEPTH, RET_HEADS * RET_DV), 0.02),
        'w_branch': nrm(ks[9], (DEPTH, N_BRANCH, BRANCH_W, D), BRANCH_W ** -0.5),
        'w_o': nrm(ks[10], (DEPTH, D, D), D ** -0.5 * DEEPNORM_BETA),
        'ln1_g': 1.0 + nrm(ks[11], (DEPTH, D), 0.02),
        'ln1_b': nrm(ks[12], (DEPTH, D), 0.02),
        'w_rg': nrm(ks[13], (DEPTH, D, N_GROUPS), D ** -0.5),
        'b_rg': nrm(ks[14], (DEPTH, N_GROUPS), 0.01),
        'w_re': nrm(ks[15], (DEPTH, D, N_EXPERTS), D ** -0.5),
        'b_re': nrm(ks[16], (DEPTH, N_EXPERTS), 0.01),
        'w1': nrm(ks[17], (DEPTH, N_EXPERTS, D, D_FF_EXPERT), D ** -0.5),
        'w3': nrm(ks[18], (DEPTH, N_EXPERTS, D, D_FF_EXPERT), D ** -0.5),
        'w2': nrm(ks[19], (DEPTH, N_EXPERTS, D_FF_EXPERT, D), D_FF_EXPERT ** -0.5 * DEEPNORM_BETA),
        'ln2_g': 1.0 + nrm(ks[20], (DEPTH, D), 0.02),
        'ln2_b': nrm(ks[21], (DEPTH, D), 0.02),
    }


def reference(x, mem, w_in, b_gate, w_mem_kv, da_lambda, da_norm_g, ret_gn_g, ret_gn_b,
              w_branch, w_o, ln1_g, ln1_b, w_rg, b_rg, w_re, b_re, w1, w3, w2, ln2_g, ln2_b):
    B, S, D = x.shape
    M = mem.shape[1]
    for l in range(DEPTH):
        lambda_init = 0.8 - 0.6 * math.exp(-0.3 * l)
        proj = x @ w_in[l]
        da_q, da_k, da_v, r_q, r_k, r_v, r_g, m_q, gate_logits = jnp.split(proj, SPLIT_POINTS, axis=-1)

        y_da = diff_attention(da_q.reshape(B, S, DA_HEADS, 2, DA_DK),
                              da_k.reshape(B, S, DA_HEADS, 2, DA_DK),
                              da_v.reshape(B, S, DA_HEADS, DA_DV),
                              da_lambda[l], da_norm_g[l], lambda_init).astype(x.dtype)

        y_ret = retention(r_q.reshape(B, S, RET_HEADS, RET_DK),
                          r_k.reshape(B, S, RET_HEADS, RET_DK),
                          r_v.reshape(B, S, RET_HEADS, RET_DV),
                          ret_gn_g[l], ret_gn_b[l])
        y_ret = (jax.nn.silu(r_g.astype(jnp.float32)) * y_ret).astype(x.dtype)

        m_k, m_v = jnp.split(mem @ w_mem_kv[l], 2, axis=-1)
        y_mem = memory_attention(m_q.reshape(B, S, MEM_HEADS, MEM_DH),
                                 m_k.reshape(B, M, MEM_HEADS, MEM_DH),
                                 m_v.reshape(B, M, MEM_HEADS, MEM_DH)).astype(x.dtype)

        gates = jax.nn.sigmoid(gate_logits + b_gate[l]).reshape(B, S, N_BRANCH, D)
        branches = jnp.stack([y_da, y_ret, y_mem], axis=2)
        branch_d = jnp.einsum('bsnc,ncd->bsnd', branches, w_branch[l])
        merged = jnp.sum(gates * branch_d, axis=2)
        x = layer_norm(DEEPNORM_ALPHA * x + merged @ w_o[l], ln1_g[l], ln1_b[l])

        y_moe = hier_moe(x, w_rg[l], b_rg[l], w_re[l], b_re[l], w1[l], w3[l], w2[l])
        x = layer_norm(DEEPNORM_ALPHA * x + y_moe, ln2_g[l], ln2_b[l])
    return x
```

```python
import contextlib
import math
import numpy as np
import concourse.bass as bass
import concourse.mybir as mybir
from concourse.bass_utils import run_bass_kernel_spmd

F32 = mybir.dt.float32
BF16 = mybir.dt.bfloat16
AF = mybir.ActivationFunctionType
ALU = mybir.AluOpType
AX = mybir.AxisListType

S5LVL = 9
S = 2048
D = 1024
NT = 16
ALPHA = 2.0 ** 0.25
EPS = 1e-5
LAMBDA_INIT = 0.8 - 0.6 * math.exp(-0.3 * 0)
GAMMA = [1.0 - 2.0 ** (-5.0 - h) for h in range(4)]
SLOPE = [2.0 ** (-(h + 1)) for h in range(8)]
MASKNEG = -float(2 ** 20)

C_ID, C_MASK, C_MRET, C_DQ, C_KDEC, C_NH, C_KD2, C_N = 0, 128, 256, 768, 1280, 1284, 1288, 1292


class Counter:
    def __init__(self, sem, step):
        self.sem = sem
        self.step = step
        self.n = 0


class Buf:
    def __init__(self, name, counter=None):
        self.name = name
        self.counter = counter
        self.w = {}
        self.r = {}

    def _keys(self, key):
        if key is None:
            return set(self.w) | set(self.r) | {None}
        return {key, None}

    def raw(self, key):
        return [self.w[k] for k in self._keys(key) if k in self.w]

    def war(self, key):
        out = []
        for k in self._keys(key):
            out += self.r.get(k, [])
        return out

    def add_read(self, key, tok):
        lst = self.r.setdefault(key, [])
        lst.append(tok)
        if len(lst) > 24:
            best = {}
            for c, v in lst:
                if best.get(c, 0) < v:
                    best[c] = v
            self.r[key] = list(best.items())

    def set_write(self, key, tok):
        if key is None:
            self.w = {None: tok}
            self.r = {}
        else:
            self.w[key] = tok
            self.r[key] = []

    def all_tokens(self):
        out = list(self.w.values())
        for v in self.r.values():
            out += v
        return out

    def absorb(self, other):
        best = {}
        for (c, v) in other.all_tokens():
            if best.get(c, 0) < v:
                best[c] = v
        lst = self.r.setdefault(None, [])
        for c, v in best.items():
            lst.append((c, v))


class Eng:
    def __init__(self, name, eng, counter, is_pe=False):
        self.name = name
        self.eng = eng
        self.counter = counter
        self.seen = {}
        self.is_pe = is_pe
        self.nwait = 0
        self.nins = 0


class FW:
    def __init__(self, nc, es):
        self.nc = nc
        self.es = es
        self.nsem = 0
        self.PE = Eng("pe", nc.tensor, self.counter("pe", 1), is_pe=True)
        self.ACT = Eng("act", nc.scalar, self.counter("act", 1))
        self.DVE = Eng("dve", nc.vector, self.counter("dve", 1))
        self.POOL = Eng("pool", nc.gpsimd, self.counter("pool", 1))
        self.SP = Eng("sp", nc.sync, self.counter("sp", 1))

    def counter(self, name, step):
        sem = self.es.enter_context(self.nc.semaphore("s_" + name))
        self.nsem += 1
        return Counter(sem, step)

    def sbuf(self, name, shape, dt, dma=False):
        t = self.es.enter_context(self.nc.sbuf_tensor("sb_" + name, shape, dt))
        b = Buf(name, self.counter("d_" + name, 16) if dma else None)
        return t, b

    def psum(self, name, shape, dt):
        t = self.es.enter_context(self.nc.psum_tensor("pp_" + name, shape, dt))
        return t, Buf(name)

    def _wait(self, E, deps):
        need = {}
        for (c, v) in deps:
            if need.get(c, 0) < v:
                need[c] = v
        for c, v in need.items():
            if E.seen.get(c, 0) >= v:
                continue
            E.eng.wait_ge(c.sem, v)
            E.seen[c] = v
            E.nwait += 1

    def op(self, E, fn, reads=(), writes=(), inc=True):
        deps = []
        own = E.counter
        for b, k in reads:
            deps += b.raw(k)
        if not E.is_pe:
            for b, k in writes:
                deps += [t for t in (b.raw(k) + b.war(k)) if t[0] is not own]
        else:
            for b, k in writes:
                deps += b.raw(k) + b.war(k)
            deps = [d for d in deps if d[0] is not own]
        self._wait(E, deps)
        ins = fn()
        E.nins += 1
        if inc:
            own.n += 1
            ins.then_inc(own.sem, 1)
            tok = (own, own.n)
        else:
            tok = (own, own.n + 1)
        for b, k in reads:
            b.add_read(k, tok)
        for b, k in writes:
            b.set_write(k, tok)
        return ins

    def dma(self, E, out, in_, reads=(), writes=(), counter=None, **kw):
        deps = []
        for b, k in reads:
            deps += b.raw(k)
        for b, k in writes:
            deps += b.raw(k) + b.war(k)
        self._wait(E, deps)
        ins = E.eng.dma_start(out=out, in_=in_, **kw)
        counter.n += 16
        ins.then_inc(counter.sem, 16)
        tok = (counter, counter.n)
        for b, k in reads:
            b.add_read(k, tok)
        for b, k in writes:
            b.set_write(k, tok)
        return ins


def build(nb=2, stop_after=None, dbg=False):
    nc = bass.Bass("TRN2", target_bir_lowering=False)

    def din(name, shape):
        return nc.dram_tensor(name, shape, F32, kind="ExternalInput").ap()

    x_d = din("x", [nb, S, D])
    mem_d = din("mem", [nb, 256, D])
    w_in_d = din("w_in", [D, 10240])
    w_kv_d = din("w_mem_kv", [D, 2048])
    w_br_d = din("w_branch", [3, D, D])
    w_o_d = din("w_o", [D, D])
    w1_d = din("w1", [16, D, 512])
    w3_d = din("w3", [16, D, 512])
    w2_d = din("w2", [16, 512, D])
    cst_d = din("cst", [128, C_N])
    aug_d = din("aug", [2, 4, S])
    pv_d = din("pv", [128, 41])
    wr_d = din("wr", [128, 160])
    bc_d = din("bc", [1, 4 * D + 20 + 256])
    out_d = nc.dram_tensor("out", [nb, S, D], F32, kind="ExternalOutput").ap()
    if dbg:
        dbg_d = nc.dram_tensor("dbg", [128, 8 * S], F32, kind="ExternalOutput").ap()

    es = contextlib.ExitStack()
    with es:
        fw = FW(nc, es)
        PE, ACT, DVE, POOL, SP = fw.PE, fw.ACT, fw.DVE, fw.POOL, fw.SP

        cst, bcst = fw.sbuf("cst", [128, C_N], F32, dma=True)
        idb, bidb = fw.sbuf("idb", [128, 128], BF16)
        maskb, bmaskb = fw.sbuf("maskb", [128, 128], BF16)
        pv, bpv = fw.sbuf("pv", [128, 48], F32, dma=True)
        wr, bwr = fw.sbuf("wr", [128, 160], F32, dma=True)
        brb, bbrb = fw.sbuf("brb", [128, 20], F32, dma=True)
        lvb, blvb = fw.sbuf("lvb", [128, 256], F32, dma=True)
        lamt, blamt = fw.sbuf("lamt", [128, 16], F32)
        lnb, blnb = fw.sbuf("lnb", [128, 2, D], F32, dma=True)
        xT, bxT = fw.sbuf("xT", [128, 8, S], BF16)
        comb, bcomb = fw.sbuf("comb", [128, NT, 16], F32)
        NSLOT = 3
        wslots = []
        for i in range(NSLOT):
            wslots.append(fw.sbuf("wslot%d" % i, [128, 4096], BF16, dma=True))
        PT = [fw.sbuf("pt%d" % i, [128, 512], BF16) for i in range(4)]
        xst = [fw.sbuf("xst%d" % i, [128, D], BF16, dma=True) for i in range(2)]
        ARENA_B = 104 * 1024
        arena, _ = fw.sbuf("arena", [128, ARENA_B // 2], BF16)
        TA_B = 19 * 1024
        tarena, _ = fw.sbuf("tarena", [128, TA_B // 2], BF16)
        PS = [fw.psum("ps%d" % i, [128, 512], F32) for i in range(8)]

        out_counters = [fw.counter("outc%d" % i, 16) for i in range(2)]

        live = {id(arena): [], id(tarena): []}

        def carve(ar, off, shape, dt, name):
            esz = 4 if dt == F32 else 2
            n = 1
            for s_ in shape[1:]:
                n *= s_
            nbytes = n * esz
            assert off % 4 == 0
            lim = ARENA_B if ar is arena else TA_B
            assert off + nbytes <= lim, (name, off, nbytes, lim)
            ap = ar[:, off // 2:(off + nbytes) // 2]
            if dt == F32:
                ap = ap.bitcast(F32)
            if len(shape) == 3:
                ap = ap.rearrange("p (a b) -> p a b", a=shape[1])
            elif len(shape) == 4:
                ap = ap.rearrange("p (a b c) -> p a b c", a=shape[1], b=shape[2])
            b = Buf(name)
            lst = live[id(ar)]
            keep = []
            for (lo, hi, ob) in lst:
                if lo < off + nbytes and off < hi:
                    b.absorb(ob)
                    if lo < off or hi > off + nbytes:
                        keep.append((lo, hi, ob))
                else:
                    keep.append((lo, hi, ob))
            keep.append((off, off + nbytes, b))
            live[id(ar)] = keep
            return ap, b

        KB = 1024
        RB0, RA1, RA2 = 0, 40 * KB, 72 * KB

        bank_rr = [0]

        def pbank(lst):
            b = lst[bank_rr[0] % len(lst)]
            bank_rr[0] += 1
            return b

        def copy(E, out, in_, reads, writes, scale=None):
            if E is ACT:
                if scale is None:
                    return fw.op(ACT, lambda: nc.scalar.copy(out, in_), reads=reads, writes=writes)
                return fw.op(ACT, lambda: nc.scalar.activation(out=out, in_=in_, func=AF.Copy, scale=scale), reads=reads, writes=writes)
            if scale is None:
                return fw.op(DVE, lambda: nc.vector.tensor_copy(out, in_), reads=reads, writes=writes)
            return fw.op(DVE, lambda: nc.vector.tensor_scalar(out=out, in0=in_, scalar1=scale, scalar2=None, op0=ALU.mult), reads=reads, writes=writes)

        alt = [0]

        def alt_eng():
            alt[0] += 1
            return ACT if alt[0] % 2 else DVE

        def mm(out, lhsT, rhs, start, stop, reads, writes, inc=None, sgc=False):
            return fw.op(PE, lambda: nc.tensor.matmul(out, lhsT=lhsT, rhs=rhs, start=start, stop=stop, skip_group_check=sgc),
                         reads=reads, writes=writes, inc=(stop if inc is None else inc))

        def tr(out, in_, ident, reads, writes, inc):
            return fw.op(PE, lambda: nc.tensor.transpose(out, in_, ident), reads=reads, writes=writes, inc=inc)

        wq = {"slots": list(wslots), "next": 0, "fifo": []}

        def w_load(pieces):
            sl = wq["slots"][wq["next"] % len(wq["slots"])]
            wq["next"] += 1
            t, b = sl
            for (off, src, k, n) in pieces:
                dst = t[:, off:off + k * n].rearrange("p (k n) -> p k n", k=k)
                fw.dma(POOL, dst, src.rearrange("(k p) n -> p k n", p=128), writes=[(b, None)], counter=b.counter)
            return sl

        def w_hint(key, pieces):
            wq["fifo"].append((key, w_load(pieces)))

        def w_get(key, pieces):
            if wq["fifo"] and wq["fifo"][0][0] == key:
                return wq["fifo"].pop(0)[1]
            assert not wq["fifo"], (key, wq["fifo"][0][0])
            return w_load(pieces)

        nxt = [None]

        def fire_next():
            if nxt[0] is not None:
                for j in nxt[0]:
                    w_hint(*j)
                nxt[0] = None

        def run_jobs(jobs, fn, fire=True):
            for i, (key, pieces) in enumerate(jobs):
                w = w_get(key, pieces)
                if i + 1 < len(jobs):
                    w_hint(*jobs[i + 1])
                elif fire:
                    fire_next()
                fn(i, w)

        sm_off = [0]

        def smalloc(n, name):
            o = sm_off[0]
            sm_off[0] += n
            assert sm_off[0] <= 256
            return sm[:, o:o + n], Buf(name)

        fw.dma(SP, cst[:], cst_d[:, :], writes=[(bcst, None)], counter=bcst.counter)
        fw.dma(SP, pv[:, 0:41], pv_d[:, :], writes=[(bpv, None)], counter=bpv.counter)
        fw.dma(SP, wr[:], wr_d[:, :], writes=[(bwr, None)], counter=bwr.counter)
        fw.dma(SP, brb[:], bc_d[:, 4 * D:4 * D + 20].partition_broadcast(128), writes=[(bbrb, None)], counter=bbrb.counter)
        fw.dma(SP, lvb[:], bc_d[:, 4 * D + 20:4 * D + 276].partition_broadcast(128), writes=[(blvb, None)], counter=blvb.counter)
        fw.op(DVE, lambda: nc.vector.tensor_copy(idb[:], cst[:, C_ID:C_ID + 128]), reads=[(bcst, None)], writes=[(bidb, None)])
        fw.op(DVE, lambda: nc.vector.tensor_copy(maskb[:], cst[:, C_MASK:C_MASK + 128]), reads=[(bcst, None)], writes=[(bmaskb, None)])
        wrh, bwrh = fw.sbuf("wrh", [128, 160], BF16)
        wrl, bwrl = fw.sbuf("wrl", [128, 160], BF16)
        fw.op(DVE, lambda: nc.vector.tensor_copy(wrh[:], wr[:]), reads=[(bwr, None)], writes=[(bwrh, None)])
        fw.op(DVE, lambda: nc.vector.tensor_tensor(out=wrl[:], in0=wr[:], in1=wrh[:], op=ALU.subtract), reads=[(bwr, None), (bwrh, None)], writes=[(bwrl, None)])
        idf = cst[:, C_ID:C_ID + 128]
        nhalf = cst[:, C_NH:C_NH + 1]
        fw.op(DVE, lambda: nc.vector.tensor_scalar(out=pv[:, 0:40], in0=pv[:, 0:40], scalar1=0.5, scalar2=None, op0=ALU.mult), reads=[(bpv, None)], writes=[(bpv, None)])
        fw.op(DVE, lambda: nc.vector.tensor_scalar(out=pv[:, 40:41], in0=pv[:, 40:41], scalar1=(1.0 - LAMBDA_INIT), scalar2=None, op0=ALU.mult), reads=[(bpv, None)], writes=[(bpv, None)])
        fw.op(DVE, lambda: nc.vector.memset(lamt[:], 0.0), writes=[(blamt, None)])
        lv4 = lvb[:].rearrange("p (a b) -> p a b", a=4)
        lprod, blprod = fw.sbuf("lprod", [128, 2, 64], F32)
        fw.op(DVE, lambda: nc.vector.tensor_tensor(out=lprod[:, 0, :], in0=lv4[:, 0, :], in1=lv4[:, 1, :], op=ALU.mult), reads=[(blvb, None)], writes=[(blprod, None)])
        fw.op(DVE, lambda: nc.vector.tensor_tensor(out=lprod[:, 1, :], in0=lv4[:, 2, :], in1=lv4[:, 3, :], op=ALU.mult), reads=[(blvb, None)], writes=[(blprod, None)])
        fw.op(DVE, lambda: nc.vector.reduce_sum(out=lamt[:, 0:2], in_=lprod[:], axis=AX.X), reads=[(blprod, None)], writes=[(blamt, None)])
        fw.op(ACT, lambda: nc.scalar.activation(out=lamt[:, 2:4], in_=lamt[:, 0:2], func=AF.Exp), reads=[(blamt, None)], writes=[(blamt, None)])
        fw.op(DVE, lambda: nc.vector.tensor_tensor(out=lamt[:, 4:5], in0=lamt[:, 2:3], in1=lamt[:, 3:4], op=ALU.subtract), reads=[(blamt, None)], writes=[(blamt, None)])
        fw.op(DVE, lambda: nc.vector.tensor_scalar(out=lamt[:, 5:6], in0=lamt[:, 4:5], scalar1=LAMBDA_INIT, scalar2=None, op0=ALU.add), reads=[(blamt, None)], writes=[(blamt, None)])
        lam = lamt[:, 5:6]

        PROJ_BANKS = [6, 7]
        ALL_BANKS = list(range(8))

        def psb(i):
            return PS[i][0], PS[i][1]

        def load_T(src_rows, ntile, dstT, bdst, keyfn):
            if ntile > 2:
                stg = [(xst[i][0][:], xst[i][1]) for i in range(2)] + [(wslots[i][0][:, 0:D], wslots[i][1]) for i in range(NSLOT)]
            else:
                stg = [(xst[i][0][:], xst[i][1]) for i in range(2)]
            LA = len(stg) - 1

            def issue(t):
                st, bst = stg[t % len(stg)]
                fw.dma(POOL, st, src_rows(t), writes=[(bst, None)], counter=bst.counter)
            for t in range(min(LA, ntile)):
                issue(t)
            for t in range(ntile):
                if t + LA < ntile:
                    issue(t + LA)
                st, bst = stg[t % len(stg)]
                bk = pbank(PROJ_BANKS)
                pt_, bpt = psb(bk)
                pb = pt_[:].bitcast(BF16)
                for c in range(8):
                    tr(pb[:, c * 128:(c + 1) * 128], st[:, c * 128:(c + 1) * 128], idb[:],
                       reads=[(bst, None), (bidb, None)], writes=[(bpt, None)], inc=(c == 7))
                copy(alt_eng(), dstT[:, :, t * 128:(t + 1) * 128], pb.rearrange("p (c t) -> p c t", c=8),
                     reads=[(bpt, None)], writes=[(bdst, keyfn(t))])

        def proj_fm(bank, wt, bw, col0, ncol, rhsT, brhs, rkey, tok0, ntok, kstride):
            pt_, bpt = psb(bank)
            for k in range(8):
                mm(pt_[0:ncol, 0:ntok], wt[:, k * kstride + col0:k * kstride + col0 + ncol], rhsT[:, k, tok0:tok0 + ntok],
                   start=(k == 0), stop=(k == 7), reads=[(bw, None), (brhs, rkey)], writes=[(bpt, None)])
            return pt_, bpt

        def proj_tm(bank, wt, bw, col0, ncol, lhsTt, blhs, lkey, tok0, kstride):
            pt_, bpt = psb(bank)
            for k in range(8):
                mm(pt_[:, 0:ncol], lhsTt[:, k, tok0:tok0 + 128], wt[:, k * kstride + col0:k * kstride + col0 + ncol],
                   start=(k == 0), stop=(k == 7), reads=[(bw, None), (blhs, lkey)], writes=[(bpt, None)])
            return pt_, bpt

        deferred = []

        def run_deferred():
            while deferred:
                deferred.pop(0)()

        def stage_da(b, yT, byT):
            qA, bqA = carve(arena, RB0 + 0 * KB, [128, S], BF16, "qA")
            qB, bqB = carve(arena, RB0 + 4 * KB, [128, S], BF16, "qB")
            kA, bkA = carve(arena, RB0 + 8 * KB, [128, S], BF16, "kA")
            kB, bkB = carve(arena, RB0 + 12 * KB, [128, S], BF16, "kB")
            V1, bV1 = carve(arena, RB0 + 16 * KB, [128, NT, 4, 130], BF16, "V1")
            aa, baa = carve(tarena, 0, [128, 4, 128], F32, "aa")
            t1, bt1 = carve(tarena, 11 * KB, [128, 4, 128], F32, "t1")
            yt4 = [carve(tarena, 3 * KB + i * KB, [128, 4, 128], BF16, "yt4_%d" % i) for i in range(2)]
            rr, brr = carve(tarena, 5 * KB, [128, 16], F32, "rr")
            ss, bss = carve(tarena, 5 * KB + 64, [128, 8], F32, "ss")
            accs, baccs = carve(tarena, 6 * KB, [128, 1164], F32, "accs")
            for (t_, b_, lo, hi, ar0, qk) in ((qA, bqA, 64, 128, 64, 0), (qB, bqB, 0, 64, 0, 0), (kA, bkA, 64, 128, 64, 1), (kB, bkB, 0, 64, 0, 1)):
                fw.op(DVE, lambda: nc.vector.memset(t_[lo:hi, :], 0.0), writes=[(b_, None)])
                fw.dma(POOL, t_[ar0:ar0 + 4, :], aug_d[qk, :, :], writes=[(b_, None)], counter=augcs[(ar0 // 64) * 2 + qk])
            fw.op(DVE, lambda: nc.vector.memset(V1[:, :, :, 128:129], 1.0), writes=[(bV1, "ones")])
            ST_BANKS = [0, 1, 2]

            def acc_ap(c, ii):
                s_ = c * 4 + ii
                bk = 3 + s_ // 3
                col = (s_ % 3) * 129
                return PS[bk][0][:, col:col + 129], PS[bk][1], s_

            grp = [0]
            for hg in range(2):
                vjob = ("dav%d_%d" % (b, hg), [(0, w_in_d[:, 2048 + hg * 512:2048 + (hg + 1) * 512], 8, 512)])
                wv, bwv = w_get(*vjob)
                qk_jobs = []
                for h in range(4):
                    H = hg * 4 + h
                    qk_jobs.append(("daqk%d_%d" % (b, H), [(0, w_in_d[:, H * 128:(H + 1) * 128], 8, 128),
                                                          (1024, w_in_d[:, 1024 + H * 128:1024 + (H + 1) * 128], 8, 128)]))
                w_hint(*qk_jobs[0])
                for t in range(NT):
                    pt_, bpt = proj_tm(pbank(PROJ_BANKS), wv, bwv, 0, 512, xT, bxT, t // 4, t * 128, 512)
                    copy(alt_eng(), V1[:, t, :, 0:128], pt_[:, :].rearrange("p (h e) -> p h e", h=4),
                         reads=[(bpt, None)], writes=[(bV1, t)])
                for h in range(4):
                    H = hg * 4 + h
                    wqk, bwqk = w_get(*qk_jobs[h])
                    if h + 1 < 4:
                        w_hint(*qk_jobs[h + 1])
                    elif hg == 0:
                        w_hint("dav%d_%d" % (b, 1), [(0, w_in_d[:, 2048 + 512:2048 + 1024], 8, 512)])
                    else:
                        fire_next()
                    qs = 2.0 ** (H - 2)
                    for tg in range(4):
                        pt_, bpt = proj_fm(pbank(PROJ_BANKS), wqk, bwqk, 0, 128, xT, bxT, tg, tg * 512, 512, 128)
                        copy(ACT, qA[0:64, tg * 512:(tg + 1) * 512], pt_[0:64, :], reads=[(bpt, None)], writes=[(bqA, tg)], scale=qs)
                        copy(DVE, qB[64:128, tg * 512:(tg + 1) * 512], pt_[64:128, :], reads=[(bpt, None)], writes=[(bqB, tg)], scale=qs)
                        pt_, bpt = proj_fm(pbank(PROJ_BANKS), wqk, bwqk, 1024, 128, xT, bxT, tg, tg * 512, 512, 128)
                        copy(ACT, kA[0:64, tg * 512:(tg + 1) * 512], pt_[0:64, :], reads=[(bpt, None)], writes=[(bkA, tg)])
                        copy(DVE, kB[64:128, tg * 512:(tg + 1) * 512], pt_[64:128, :], reads=[(bpt, None)], writes=[(bkB, tg)])
                    pend = []

                    def epilogue(I):
                            run_deferred()
                            yt, byt = yt4[(H * 4 + I) % 2]
                            for bk3 in range(3):
                                ncp = 387 if bk3 < 2 else 258
                                fw.op(DVE, lambda: nc.vector.tensor_copy(accs[:, bk3 * 387:bk3 * 387 + ncp], PS[3 + bk3][0][:, 0:ncp]),
                                      reads=[(PS[3 + bk3][1], None)], writes=[(baccs, bk3)])
                            A4 = accs[:, 0:1032].rearrange("p (c i e) -> p c i e", c=2, i=4)
                            RA = [(baccs, None)]
                            fw.op(DVE, lambda: nc.vector.reciprocal(rr[:, 0:8].rearrange("p (c i) -> p c i", c=2), A4[:, :, :, 128]), reads=RA, writes=[(brr, None)])
                            fw.op(DVE, lambda: nc.vector.tensor_scalar(out=rr[:, 8:12], in0=rr[:, 4:8], scalar1=lam, scalar2=None, op0=ALU.mult),
                                  reads=[(brr, None), (blamt, None)], writes=[(brr, None)])
                            fw.op(DVE, lambda: nc.vector.tensor_tensor(out=t1[:], in0=A4[:, 1, :, 0:128], in1=rr[:, 8:12].unsqueeze(2).broadcast_to([128, 4, 128]), op=ALU.mult),
                                  reads=RA + [(brr, None)], writes=[(bt1, None)])
                            fw.op(DVE, lambda: nc.vector.tensor_tensor(out=aa[:], in0=A4[:, 0, :, 0:128], in1=rr[:, 0:4].unsqueeze(2).broadcast_to([128, 4, 128]), op=ALU.mult),
                                  reads=RA + [(brr, None)], writes=[(baa, None)])
                            fw.op(DVE, lambda: nc.vector.tensor_tensor(out=aa[:], in0=aa[:], in1=t1[:], op=ALU.subtract), reads=[(baa, None), (bt1, None)], writes=[(baa, None)])
                            fw.op(DVE, lambda: nc.vector.tensor_tensor(out=t1[:], in0=aa[:], in1=aa[:], op=ALU.mult), reads=[(baa, None)], writes=[(bt1, None)])
                            fw.op(DVE, lambda: nc.vector.reduce_sum(out=ss[:, 0:4], in_=t1[:], axis=AX.X), reads=[(bt1, None)], writes=[(bss, None)])
                            fw.op(POOL, lambda: nc.gpsimd.tensor_scalar(out=ss[:, 4:8], in0=ss[:, 0:4], scalar1=1.0 / 128.0, scalar2=EPS, op0=ALU.mult, op1=ALU.add),
                                  reads=[(bss, None)], writes=[(bss, None)])
                            fw.op(POOL, lambda: nc.gpsimd.tensor_tensor(out=ss[:, 0:4], in0=ss[:, 4:8], in1=nhalf.broadcast_to([128, 4]), op=ALU.pow),
                                  reads=[(bss, None), (bcst, None)], writes=[(bss, None)])
                            fw.op(DVE, lambda: nc.vector.tensor_tensor(out=yt[:], in0=aa[:], in1=ss[:, 0:4].unsqueeze(2).broadcast_to([128, 4, 128]), op=ALU.mult),
                                  reads=[(baa, None), (bss, None)], writes=[(byt, None)])

                            def part2(yt=yt, byt=byt, H=H, I=I):
                                bk = pbank(PROJ_BANKS)
                                pt_, bpt = psb(bk)
                                pb = pt_[:].bitcast(BF16)
                                for ii in range(4):
                                    tr(pb[:, ii * 128:(ii + 1) * 128], yt[:, ii, :], idb[:], reads=[(byt, None), (bidb, None)], writes=[(bpt, None)], inc=(ii == 3))
                                fw.op(DVE, lambda: nc.vector.tensor_scalar(out=yT[:, H, I * 512:(I + 1) * 512], in0=pb[:, 0:512], scalar1=pv[:, 40:41], scalar2=None, op0=ALU.mult),
                                      reads=[(bpt, None), (bpv, None)], writes=[(byT, (H, I))])
                            deferred.append(part2)

                    def do_av(st):
                        (I, c, j, i0, ncols, pslot) = st
                        ptt, bptt = PT[pslot]
                        for i in range(i0, 4 * I + 4):
                            ii = i - 4 * I
                            ap_, bacc, s_ = acc_ap(c, ii)
                            mm(ap_, ptt[:, (i - i0) * 128:(i - i0 + 1) * 128], V1[:, j, h, 0:129],
                               start=(j == 0 and s_ % 3 == 0), stop=(j == i), reads=[(bptt, None), (bV1, None)], writes=[(bacc, s_)], inc=(j == i or i == 4 * I + 3), sgc=True)
                        if c == 1 and j == 4 * I + 3:
                            epilogue(I)

                    for I in range(4):
                        for c in range(2):
                            qX, bqX, kX, bkX = (qA, bqA, kA, bkA) if c == 0 else (qB, bqB, kB, bkB)
                            for j in range(4 * I + 4):
                                i0 = max(4 * I, j)
                                ncols = (4 * I + 4 - i0) * 128
                                diag = (j >= 4 * I)
                                stb = pbank(ST_BANKS)
                                pst, bpst = psb(stb)
                                mm(pst[:, 0:ncols], kX[:, j * 128:(j + 1) * 128], qX[:, i0 * 128:i0 * 128 + ncols],
                                   start=True, stop=(not diag), reads=[(bkX, None), (bqX, None)], writes=[(bpst, None)])
                                if diag:
                                    mm(pst[:, 0:128], idb[:], maskb[:], start=False, stop=True,
                                       reads=[(bidb, None), (bmaskb, None)], writes=[(bpst, None)])
                                pslot = grp[0] % 4
                                grp[0] += 1
                                ptt, bptt = PT[pslot]
                                fw.op(ACT, lambda: nc.scalar.activation(out=ptt[:, 0:ncols], in_=pst[:, 0:ncols], func=AF.Exp, scale=SLOPE[H]),
                                      reads=[(bpst, None)], writes=[(bptt, None)])
                                pend.append((I, c, j, i0, ncols, pslot))
                                if len(pend) > 2:
                                    do_av(pend.pop(0))
                    while pend:
                        do_av(pend.pop(0))
            run_deferred()

        def stage_ret(b, yT, byT):
            st_f, bst_f = carve(tarena, 0, [128, 2, 256], F32, "st_f")
            st_b, bst_b = carve(tarena, 2 * KB, [128, 2, 256], BF16, "st_b")
            stm = [carve(tarena, 3 * KB + i * 256, [128, 128], BF16, "stm%d" % i) for i in range(4)]
            zt, bzt = carve(tarena, 4 * KB, [128, 2, 4, 512], BF16, "zt")
            zt = zt.rearrange("p g q (h e) -> p g q h e", h=2)
            thb = [carve(tarena, 12 * KB + i * KB, [128, 512], BF16, "thb%d" % i) for i in range(2)]
            sgb = [carve(tarena, 14 * KB + i * KB, [128, 512], BF16, "sgb%d" % i) for i in range(2)]
            yzb = [carve(tarena, 16 * KB + i * KB, [128, 512], BF16, "yzb%d" % i) for i in range(2)]
            bns, bbns = carve(tarena, 18 * KB, [128, 2, 8], F32, "bns")
            mv, bmv = carve(tarena, 18 * KB + 64, [128, 4, 4], F32, "mv")
            mv = mv.rearrange("p (a h) c -> p a h c", a=2)
            for hp in range(2):
                qT = [carve(arena, RB0 + i * 4 * KB, [128, S], BF16, "rq%d" % i) for i in range(2)]
                kT = [carve(arena, RB0 + 8 * KB + i * 4 * KB, [128, S], BF16, "rk%d" % i) for i in range(2)]
                kt = [carve(arena, RB0 + 16 * KB + i * 4 * KB, [128, NT, 128], BF16, "rkt%d" % i) for i in range(2)]
                vv, bvv = carve(arena, RB0 + 24 * KB, [128, NT, 2, 256], BF16, "rv")
                c0 = 3072 + hp * 256
                c1 = 3584 + hp * 256
                wqk, bwqk = w_get("rqk%d_%d" % (b, hp), [(0, w_in_d[:, c0:c0 + 256], 8, 256), (2048, w_in_d[:, c1:c1 + 256], 8, 256)])
                w_hint("rv%d_%d" % (b, hp), [(0, w_in_d[:, 4096 + hp * 512:4096 + (hp + 1) * 512], 8, 512)])
                for hh in range(2):
                    H = hp * 2 + hh
                    for tg in range(4):
                        pt_, bpt = proj_fm(pbank(ALL_BANKS), wqk, bwqk, hh * 128, 128, xT, bxT, tg, tg * 512, 512, 256)
                        fw.op(DVE, lambda: nc.vector.tensor_tensor(out=qT[hh][0][:, tg * 512:(tg + 1) * 512].rearrange("p (a q) -> p a q", a=4),
                                                                   in0=pt_[:, :].rearrange("p (a q) -> p a q", a=4),
                                                                   in1=cst[:, C_DQ + H * 128:C_DQ + (H + 1) * 128].unsqueeze(1).broadcast_to([128, 4, 128]),
                                                                   op=ALU.mult),
                              reads=[(bpt, None), (bcst, None)], writes=[(qT[hh][1], tg)])
                        pt_, bpt = proj_fm(pbank(ALL_BANKS), wqk, bwqk, 2048 + hh * 128, 128, xT, bxT, tg, tg * 512, 512, 256)
                        copy(ACT, kT[hh][0][:, tg * 512:(tg + 1) * 512], pt_[:, :], reads=[(bpt, None)], writes=[(kT[hh][1], tg)], scale=128.0 ** -0.5)
                for t4 in range(NT // 4):
                    pt_, bpt = psb(pbank(ALL_BANKS))
                    pbv = pt_[:].bitcast(BF16).rearrange("p (t h d) -> p t h d", t=4, h=2)
                    for q4 in range(4):
                        t = t4 * 4 + q4
                        for hh in range(2):
                            tr(pbv[:, q4, hh, :], kT[hh][0][:, t * 128:(t + 1) * 128], idb[:],
                               reads=[(kT[hh][1], t4), (bidb, None)], writes=[(bpt, None)], inc=(q4 == 3 and hh == 1))
                    for hh in range(2):
                        H = hp * 2 + hh
                        fw.op(ACT, lambda: nc.scalar.activation(out=kt[hh][0][:, t4 * 4:(t4 + 1) * 4, :], in_=pbv[:, :, hh, :], func=AF.Identity,
                                                                scale=cst[:, C_KD2 + H:C_KD2 + H + 1]),
                              reads=[(bpt, None), (bcst, None)], writes=[(kt[hh][1], t4)])
                wv, bwv = w_get("rv%d_%d" % (b, hp), None)
                w_hint("rg%d_%d" % (b, hp), [(0, w_in_d[:, 5120 + hp * 512:5120 + (hp + 1) * 512], 8, 512)])
                for t in range(NT):
                    pt_, bpt = proj_tm(pbank(ALL_BANKS), wv, bwv, 0, 512, xT, bxT, t // 4, t * 128, 512)
                    copy(alt_eng(), vv[:, t, :, :], pt_[:, :].rearrange("p (h e) -> p h e", h=2), reads=[(bpt, None)], writes=[(bvv, t)])
                wg, bwg = w_get("rg%d_%d" % (b, hp), None)
                if hp == 1:
                    fire_next()
                if hp == 0:
                    c0n = 3072 + 256
                    c1n = 3584 + 256
                    w_hint("rqk%d_%d" % (b, 1), [(0, w_in_d[:, c0n:c0n + 256], 8, 256), (2048, w_in_d[:, c1n:c1n + 256], 8, 256)])
                RB_A = [0, 1, 6, 7]

                def emit_scores(n):
                    cs = slice(n * 128, (n + 1) * 128)
                    for hh in range(2):
                        H = hp * 2 + hh
                        ps_, bps = psb(pbank(RB_A))
                        mm(ps_[:, 0:128], kT[hh][0][:, cs], qT[hh][0][:, cs], start=True, stop=True,
                           reads=[(kT[hh][1], n // 4), (qT[hh][1], n // 4)], writes=[(bps, None)])
                        sm_, bsm_ = stm[(n % 2) * 2 + hh]
                        fw.op(DVE, lambda: nc.vector.tensor_tensor(out=sm_[:], in0=ps_[:, 0:128], in1=cst[:, C_MRET + H * 128:C_MRET + (H + 1) * 128], op=ALU.mult),
                              reads=[(bps, None), (bcst, None)], writes=[(bsm_, None)])

                def emit_z(n):
                    for hh in range(2):
                        po_, bpo = psb(2 + (n % 2) * 2 + hh)
                        fw.op(DVE, lambda: nc.vector.tensor_scalar(out=zt[:, (n // 4) % 2, n % 4, hh, :], in0=po_[:, 0:256], scalar1=mv[:, n % 2, hh, 0:1], scalar2=mv[:, n % 2, hh, 3:4],
                                                                   op0=ALU.subtract, op1=ALU.mult),
                              reads=[(bpo, None), (bmv, (n % 2, hh))], writes=[(bzt, ((n // 4) % 2, n % 4, hh))])

                def emit_group(tg):
                    for hh in range(2):
                        H = hp * 2 + hh
                        for ec in range(2):
                            ch = H * 2 + ec
                            pt_, bpt = psb(pbank(RB_A))
                            pb = pt_[:].bitcast(BF16)
                            for q4 in range(4):
                                tr(pb[:, q4 * 128:(q4 + 1) * 128], zt[:, tg % 2, q4, hh, ec * 128:(ec + 1) * 128], idb[:],
                                   reads=[(bzt, (tg % 2, q4, hh)), (bidb, None)], writes=[(bpt, None)], inc=(q4 == 3))
                            pg_, bpg = proj_fm(pbank(RB_A), wg, bwg, hh * 256 + ec * 128, 128, xT, bxT, tg, tg * 512, 512, 512)
                            th_, bth = thb[ec]
                            sg_, bsg = sgb[ec]
                            yz_, byz = yzb[ec]
                            fw.op(ACT, lambda: nc.scalar.activation(out=th_[:], in_=pg_[:, :], func=AF.Tanh, scale=0.5), reads=[(bpg, None)], writes=[(bth, None)])
                            fw.op(DVE, lambda: nc.vector.scalar_tensor_tensor(out=sg_[:], in0=th_[:], scalar=1.0, in1=pg_[:, :], op0=ALU.add, op1=ALU.mult),
                                  reads=[(bth, None), (bpg, None)], writes=[(bsg, None)])
                            fw.op(ACT, lambda: nc.scalar.activation(out=yz_[:], in_=pb[:, 0:512], func=AF.Identity, scale=pv[:, 24 + ch:25 + ch], bias=pv[:, 32 + ch:33 + ch]),
                                  reads=[(bpt, None), (bpv, None)], writes=[(byz, None)])
                            fw.op(DVE, lambda: nc.vector.tensor_tensor(out=yT[:, ch, tg * 512:(tg + 1) * 512], in0=yz_[:], in1=sg_[:], op=ALU.mult),
                                  reads=[(byz, None), (bsg, None)], writes=[(byT, (ch, tg))])

                emit_scores(0)
                for n in range(NT):
                    cs = slice(n * 128, (n + 1) * 128)
                    if n + 1 < NT:
                        emit_scores(n + 1)
                    for hh in range(2):
                        H = hp * 2 + hh
                        sm_, bsm_ = stm[(n % 2) * 2 + hh]
                        po_, bpo = psb(2 + (n % 2) * 2 + hh)
                        mm(po_[:, 0:256], sm_[:], vv[:, n, hh, :], start=True, stop=(n == 0), reads=[(bsm_, None), (bvv, n)], writes=[(bpo, None)])
                        if n > 0:
                            mm(po_[:, 0:256], qT[hh][0][:, cs], st_b[:, hh, :], start=False, stop=True,
                               reads=[(qT[hh][1], n // 4), (bst_b, hh)], writes=[(bpo, None)])
                    if n < NT - 1:
                        for hh in range(2):
                            H = hp * 2 + hh
                            pk_, bpk = psb(pbank(RB_A))
                            mm(pk_[:, 0:256], kt[hh][0][:, n, :], vv[:, n, hh, :], start=True, stop=True,
                               reads=[(kt[hh][1], n // 4), (bvv, n)], writes=[(bpk, None)])
                            if n == 0:
                                fw.op(DVE, lambda: nc.vector.tensor_copy(st_f[:, hh, :], pk_[:, 0:256]), reads=[(bpk, None)], writes=[(bst_f, hh)])
                            else:
                                fw.op(DVE, lambda: nc.vector.scalar_tensor_tensor(out=st_f[:, hh, :], in0=st_f[:, hh, :], scalar=GAMMA[H] ** 128, in1=pk_[:, 0:256],
                                                                                  op0=ALU.mult, op1=ALU.add),
                                      reads=[(bpk, None), (bst_f, hh)], writes=[(bst_f, hh)])
                            fw.op(ACT, lambda: nc.scalar.copy(st_b[:, hh, :], st_f[:, hh, :]), reads=[(bst_f, hh)], writes=[(bst_b, hh)])
                    for hh in range(2):
                        po_, bpo = psb(2 + (n % 2) * 2 + hh)
                        fw.op(DVE, lambda: nc.vector.bn_stats(out=bns[:, hh, 0:6], in_=po_[:, 0:256]), reads=[(bpo, None)], writes=[(bbns, hh)])
                        fw.op(DVE, lambda: nc.vector.bn_aggr(out=mv[:, n % 2, hh, 0:2], in_=bns[:, hh, 0:6]), reads=[(bbns, hh)], writes=[(bmv, (n % 2, hh))])
                        fw.op(POOL, lambda: nc.gpsimd.tensor_scalar(out=mv[:, n % 2, hh, 2:3], in0=mv[:, n % 2, hh, 1:2], scalar1=EPS, scalar2=None, op0=ALU.add),
                              reads=[(bmv, (n % 2, hh))], writes=[(bmv, (n % 2, hh))])
                        fw.op(POOL, lambda: nc.gpsimd.tensor_tensor(out=mv[:, n % 2, hh, 3:4], in0=mv[:, n % 2, hh, 2:3], in1=nhalf, op=ALU.pow),
                              reads=[(bmv, (n % 2, hh)), (bcst, None)], writes=[(bmv, (n % 2, hh))])
                    if n >= 1:
                        emit_z(n - 1)
                    if n >= 5 and (n - 5) % 4 == 0:
                        emit_group((n - 5) // 4)
                emit_z(NT - 1)
                emit_group(3)

        def stage_mem(b, yT, byT):
            memT, bmemT = carve(arena, RB0 + 0, [128, 8, 256], BF16, "memT")
            mKT, bmKT = carve(arena, RB0 + 4 * KB, [128, 8, 256], BF16, "mKT")
            mV1, bmV1 = carve(arena, RB0 + 8 * KB, [128, 2, 4, 258], BF16, "mV1")
            mq = [carve(arena, RB0 + 13 * KB + i * 8 * KB, [128, 2, S], BF16, "mq%d" % i) for i in range(2)]
            ymts = [carve(tarena, i * 2 * KB, [128, 4, 256], BF16, "ymt%d" % i) for i in range(2)]
            rr, brr = carve(tarena, 4 * KB, [128, 4], F32, "mrr")
            load_T(lambda t: mem_d[b, t * 128:(t + 1) * 128, :], 2, memT, bmemT, lambda t: None)
            fw.op(DVE, lambda: nc.vector.memset(mV1[:, :, :, 256:257], 1.0), writes=[(bmV1, "ones")])
            jobs = [("mk%d_%d" % (b, i), [(0, w_kv_d[:, i * 512:(i + 1) * 512], 8, 512)]) for i in range(4)]

            def kvjob(i, w):
                wt, bw = w
                if i == 3:
                    w_hint("mq%d_0" % b, [(0, w_in_d[:, 6144:6144 + 256], 8, 256)])
                if i < 2:
                    for cc in range(4):
                        c = i * 4 + cc
                        pt_, bpt = psb(pbank(ALL_BANKS))
                        for k in range(8):
                            mm(pt_[:, 0:256], wt[:, k * 512 + cc * 128:k * 512 + (cc + 1) * 128], memT[:, k, :], start=(k == 0), stop=(k == 7),
                               reads=[(bw, None), (bmemT, None)], writes=[(bpt, None)])
                        copy(alt_eng(), mKT[:, c, :], pt_[:, 0:256], reads=[(bpt, None)], writes=[(bmKT, c)], scale=1.0 / 16.0)
                else:
                    hf = i - 2
                    for mt in range(2):
                        pt_, bpt = psb(pbank(ALL_BANKS))
                        for k in range(8):
                            mm(pt_[:, :], memT[:, k, mt * 128:(mt + 1) * 128], wt[:, k * 512:(k + 1) * 512], start=(k == 0), stop=(k == 7),
                               reads=[(bw, None), (bmemT, None)], writes=[(bpt, None)])
                        copy(alt_eng(), mV1[:, mt, hf * 2:hf * 2 + 2, 0:256], pt_[:, :].rearrange("p (h e) -> p h e", h=2),
                             reads=[(bpt, None)], writes=[(bmV1, (mt, hf))])
            run_jobs(jobs, kvjob, fire=False)
            for H in range(4):
                wq_, bwq = w_get("mq%d_%d" % (b, H), None)
                if H < 3:
                    w_hint("mq%d_%d" % (b, H + 1), [(0, w_in_d[:, 6144 + (H + 1) * 256:6144 + (H + 2) * 256], 8, 256)])
                else:
                    fire_next()
                mqt, bmq = mq[H % 2]
                for dc in range(2):
                    for tg in range(4):
                        pt_, bpt = proj_fm(pbank([6, 7]), wq_, bwq, dc * 128, 128, xT, bxT, tg, tg * 512, 512, 256)
                        copy(alt_eng(), mqt[:, dc, tg * 512:(tg + 1) * 512], pt_[:, :], reads=[(bpt, None)], writes=[(bmq, tg)])
                for tg in range(4):
                    for mt in range(2):
                        pst, bpst = psb(pbank([4, 5]))
                        for dc in range(2):
                            mm(pst[:, :], mKT[:, H * 2 + dc, mt * 128:(mt + 1) * 128], mqt[:, dc, tg * 512:(tg + 1) * 512], start=(dc == 0), stop=(dc == 1),
                               reads=[(bmKT, None), (bmq, tg)], writes=[(bpst, None)])
                        ptt, bptt = PT[(tg * 2 + mt) % 4]
                        fw.op(ACT, lambda: nc.scalar.activation(out=ptt[:], in_=pst[:, :], func=AF.Exp), reads=[(bpst, None)], writes=[(bptt, None)])
                        for ii in range(4):
                            pa, bpa = psb(ii)
                            mm(pa[:, 0:257], ptt[:, ii * 128:(ii + 1) * 128], mV1[:, mt, H, 0:257], start=(mt == 0), stop=(mt == 1),
                               reads=[(bptt, None), (bmV1, None)], writes=[(bpa, None)], inc=True)
                    run_deferred()
                    ymt, bymt = ymts[(H * 4 + tg) % 2]
                    for ii in range(4):
                        pa, bpa = psb(ii)
                        fw.op(DVE, lambda: nc.vector.reciprocal(rr[:, ii:ii + 1], pa[:, 256:257]), reads=[(bpa, None)], writes=[(brr, ii)])
                        fw.op(DVE, lambda: nc.vector.tensor_scalar(out=ymt[:, ii, :], in0=pa[:, 0:256], scalar1=rr[:, ii:ii + 1], scalar2=None, op0=ALU.mult),
                              reads=[(bpa, None), (brr, ii)], writes=[(bymt, ii)])

                    def part2(H=H, tg=tg, ymt=ymt, bymt=bymt):
                        for ec in range(2):
                            pt_, bpt = psb(pbank([6, 7]))
                            pb = pt_[:].bitcast(BF16)
                            for ii in range(4):
                                tr(pb[:, ii * 128:(ii + 1) * 128], ymt[:, ii, ec * 128:(ec + 1) * 128], idb[:],
                                   reads=[(bymt, ii), (bidb, None)], writes=[(bpt, None)], inc=(ii == 3))
                            copy(alt_eng(), yT[:, H * 2 + ec, tg * 512:(tg + 1) * 512], pb[:, 0:512], reads=[(bpt, None)], writes=[(byT, (H * 2 + ec, tg))])
                    deferred.append(part2)
            run_deferred()

        def fold(b, n, yT, byT, mg, bmg, first):
            thb = [carve(tarena, i * KB, [128, 512], BF16, "fth%d" % i) for i in range(2)]
            tmp = [carve(tarena, 2 * KB + i * 2 * KB, [128, 512], F32, "ftmp%d" % i) for i in range(2)]
            jobs = []
            for dc in range(8):
                g0 = 7168 + n * 1024 + dc * 128
                jobs.append(("fold%d_%d_%d" % (b, n, dc), [(0, w_br_d[n][:, dc * 128:(dc + 1) * 128], 8, 128), (1024, w_in_d[:, g0:g0 + 128], 8, 128)]))

            def job(dc, w):
                wt, bw = w
                for tg in range(4):
                    pbd, bpbd = proj_fm(pbank(ALL_BANKS), wt, bw, 0, 128, yT, byT, None, tg * 512, 512, 128)
                    pg_, bpg = proj_fm(pbank(ALL_BANKS), wt, bw, 1024, 128, xT, bxT, tg, tg * 512, 512, 128)
                    th_, bth = thb[tg % 2]
                    col = n * 8 + dc
                    fw.op(ACT, lambda: nc.scalar.activation(out=th_[:], in_=pg_[:, :], func=AF.Tanh, scale=0.5, bias=pv[:, col:col + 1]),
                          reads=[(bpg, None), (bpv, None)], writes=[(bth, None)])
                    dst = mg[:, dc, tg * 512:(tg + 1) * 512]
                    if first:
                        fw.op(DVE, lambda: nc.vector.scalar_tensor_tensor(out=dst, in0=th_[:], scalar=1.0, in1=pbd[:, :], op0=ALU.add, op1=ALU.mult),
                              reads=[(bth, None), (bpbd, None)], writes=[(bmg, (dc, tg))])
                    else:
                        tm_, btm = tmp[tg % 2]
                        fw.op(DVE, lambda: nc.vector.scalar_tensor_tensor(out=tm_[:], in0=th_[:], scalar=1.0, in1=pbd[:, :], op0=ALU.add, op1=ALU.mult),
                              reads=[(bth, None), (bpbd, None)], writes=[(btm, None)])
                        fw.op(DVE, lambda: nc.vector.tensor_tensor(out=dst, in0=tm_[:], in1=dst, op=ALU.add),
                              reads=[(btm, None), (bmg, (dc, tg))], writes=[(bmg, (dc, tg))])
            run_jobs(jobs, job)

        def layer_norm(h, bh, hkey, bns, bbns, mv, bmv, out, bout, okey):
            hv = h.rearrange("p (a f) -> p a f", a=2)
            for a_ in range(2):
                fw.op(DVE, lambda: nc.vector.bn_stats(out=bns[:, a_, 0:6], in_=hv[:, a_, :]), reads=[(bh, hkey)], writes=[(bbns, a_)])
            fw.op(DVE, lambda: nc.vector.bn_aggr(out=mv[:, 0:2], in_=bns[:, :, 0:6]), reads=[(bbns, None)], writes=[(bmv, None)])
            fw.op(POOL, lambda: nc.gpsimd.tensor_scalar(out=mv[:, 2:3], in0=mv[:, 1:2], scalar1=EPS, scalar2=None, op0=ALU.add), reads=[(bmv, None)], writes=[(bmv, None)])
            fw.op(POOL, lambda: nc.gpsimd.tensor_tensor(out=mv[:, 3:4], in0=mv[:, 2:3], in1=nhalf, op=ALU.pow), reads=[(bmv, None), (bcst, None)], writes=[(bmv, None)])
            fw.op(DVE, lambda: nc.vector.tensor_scalar(out=out, in0=h, scalar1=mv[:, 0:1], scalar2=mv[:, 3:4], op0=ALU.subtract, op1=ALU.mult),
                  reads=[(bh, hkey), (bmv, None)], writes=[(bout, okey)])
            fw.op(DVE, lambda: nc.vector.tensor_tensor(out=out, in0=out, in1=lnb[:, 0, :], op=ALU.mult), reads=[(bout, okey), (blnb, None)], writes=[(bout, okey)])
            fw.op(DVE, lambda: nc.vector.tensor_tensor(out=out, in0=out, in1=lnb[:, 1, :], op=ALU.add), reads=[(bout, okey), (blnb, None)], writes=[(bout, okey)])

        def stage5(b, mg, bmg):
            acc, bacc = carve(arena, 0, [128, NT, D], F32, "acc")
            xr = [carve(tarena, i * 4 * KB, [128, D], F32, "xr%d" % i) for i in range(2)]
            hb = [carve(tarena, 8 * KB + i * 4 * KB, [128, D], F32, "hb%d" % i) for i in range(2)]
            xr.append(carve(arena, 64 * KB, [128, D], F32, "xr2"))
            hb.append(carve(arena, 68 * KB, [128, D], F32, "hb2"))
            xrc = [fw.counter("xrc%d_%d" % (b, i), 16) for i in range(3)]
            bnsl = [carve(tarena, 16 * KB + i * 64, [128, 2, 8], F32, "bns5_%d" % i) for i in range(2)]
            mvl = [carve(tarena, 16 * KB + 128 + i * 16, [128, 4], F32, "mv5_%d" % i) for i in range(2)]
            rt, brt = carve(tarena, 16 * KB + 192, [128, 600], F32, "rt")
            tlTl = [(xst[i][0][:].rearrange("p (c t) -> p c t", c=8), xst[i][1]) for i in range(2)]
            fw.dma(SP, lnb[:, 0, :], bc_d[:, 0:D].partition_broadcast(128), writes=[(blnb, None)], counter=blnb.counter)
            fw.dma(SP, lnb[:, 1, :], bc_d[:, D:2 * D].partition_broadcast(128), writes=[(blnb, None)], counter=blnb.counter)
            wo = []
            wo.append(w_get("wo%d_0" % b, [(0, w_o_d[:, 0:512], 8, 512)]))
            wo.append(w_get("wo%d_1" % b, [(0, w_o_d[:, 512:1024], 8, 512)]))
            prs = {}
            S5_BANKS = [0, 1, 2, 3, 4, 5]

            def phA(t):
                xr_, bxr = xr[t % 3]
                hb_, bhb = hb[t % 3]
                fw.dma(SP, xr_, x_d[b, t * 128:(t + 1) * 128, :], writes=[(bxr, None)], counter=xrc[t % 3])
                fw.op(ACT, lambda: nc.scalar.mul(xr_, xr_, ALPHA), reads=[(bxr, None)], writes=[(bxr, None)])
                for hf in range(2):
                    wt, bw = wo[hf]
                    pt_, bpt = proj_tm(pbank(S5_BANKS), wt, bw, 0, 512, mg, bmg, None, t * 128, 512)
                    fw.op(DVE, lambda: nc.vector.scalar_tensor_tensor(out=hb_[:, hf * 512:(hf + 1) * 512], in0=pt_[:, :], scalar=0.5, in1=xr_[:, hf * 512:(hf + 1) * 512],
                                                                      op0=ALU.mult, op1=ALU.add),
                          reads=[(bpt, None), (bxr, None)], writes=[(bhb, hf)])

            def phB1(t):
                hb_, bhb = hb[t % 3]
                bns, bbns = bnsl[t % 2]
                mv, bmv = mvl[t % 2]
                hv = hb_.rearrange("p (a f) -> p a f", a=2)
                for a_ in range(2):
                    fw.op(DVE, lambda: nc.vector.bn_stats(out=bns[:, a_, 0:6], in_=hv[:, a_, :]), reads=[(bhb, None)], writes=[(bbns, a_)])
                fw.op(DVE, lambda: nc.vector.bn_aggr(out=mv[:, 0:2], in_=bns[:, :, 0:6]), reads=[(bbns, None)], writes=[(bmv, None)])
                fw.op(POOL, lambda: nc.gpsimd.tensor_scalar(out=mv[:, 2:3], in0=mv[:, 1:2], scalar1=EPS, scalar2=None, op0=ALU.add), reads=[(bmv, None)], writes=[(bmv, None)])
                fw.op(POOL, lambda: nc.gpsimd.tensor_tensor(out=mv[:, 3:4], in0=mv[:, 2:3], in1=nhalf, op=ALU.pow), reads=[(bmv, None), (bcst, None)], writes=[(bmv, None)])

            def phB2(t):
                xr_, bxr = xr[t % 3]
                hb_, bhb = hb[t % 3]
                mv, bmv = mvl[t % 2]
                fw.op(DVE, lambda: nc.vector.tensor_scalar(out=hb_, in0=hb_, scalar1=mv[:, 0:1], scalar2=mv[:, 3:4], op0=ALU.subtract, op1=ALU.mult),
                      reads=[(bhb, None), (bmv, None)], writes=[(bhb, None)])
                fw.op(DVE, lambda: nc.vector.tensor_tensor(out=hb_, in0=hb_, in1=lnb[:, 0, :], op=ALU.mult), reads=[(bhb, None), (blnb, None)], writes=[(bhb, None)])
                fw.op(POOL, lambda: nc.gpsimd.tensor_tensor(out=hb_, in0=hb_, in1=lnb[:, 1, :], op=ALU.add), reads=[(bhb, None), (blnb, None)], writes=[(bhb, None)])
                tb = xr_.bitcast(BF16)
                thi = tb[:, 0:D]
                tlo = tb[:, D:2 * D]
                fw.op(ACT, lambda: nc.scalar.copy(thi, hb_), reads=[(bhb, None)], writes=[(bxr, None)])
                fw.op(DVE, lambda: nc.vector.tensor_tensor(out=tlo, in0=hb_, in1=thi, op=ALU.subtract), reads=[(bhb, None), (bxr, None)], writes=[(bxr, None)])

            def phC(t):
                xr_, bxr = xr[t % 3]
                hb_, bhb = hb[t % 3]
                tlT, btlT = tlTl[t % 2]
                tb = xr_.bitcast(BF16)
                thi = tb[:, 0:D]
                tlo = tb[:, D:2 * D]
                pth, bpth = psb(pbank(S5_BANKS))
                ptl, bptl = psb(pbank(S5_BANKS))
                pthb = pth[:].bitcast(BF16)
                ptlb = ptl[:].bitcast(BF16)
                for c in range(8):
                    tr(pthb[:, c * 128:(c + 1) * 128], thi[:, c * 128:(c + 1) * 128], idb[:], reads=[(bxr, None), (bidb, None)], writes=[(bpth, None)], inc=(c == 7))
                for c in range(8):
                    tr(ptlb[:, c * 128:(c + 1) * 128], tlo[:, c * 128:(c + 1) * 128], idb[:], reads=[(bxr, None), (bidb, None)], writes=[(bptl, None)], inc=(c == 7))
                fw.op(ACT, lambda: nc.scalar.copy(xT[:, :, t * 128:(t + 1) * 128], pthb.rearrange("p (c t) -> p c t", c=8)),
                      reads=[(bpth, None)], writes=[(bxT, t // 4)])
                fw.op(DVE, lambda: nc.vector.tensor_copy(tlT, ptlb.rearrange("p (c t) -> p c t", c=8)), reads=[(bptl, None)], writes=[(btlT, None)])
                if t % 4 == 0:
                    prs[t // 4] = psb(6 + (t // 4) % 2)
                pr_, bpr = prs[t // 4]
                q4 = t % 4
                nmm = 0
                for k in range(8):
                    for (lt, blt, lkey, rt_, brt_) in ((xT[:, k, t * 128:(t + 1) * 128], bxT, t // 4, wrh, bwrh),
                                                       (tlT[:, k, :], btlT, None, wrh, bwrh),
                                                       (xT[:, k, t * 128:(t + 1) * 128], bxT, t // 4, wrl, bwrl)):
                        mm(pr_[:, q4 * 20:(q4 + 1) * 20], lt, rt_[:, k * 20:(k + 1) * 20], start=(nmm == 0 and q4 == 0), stop=(nmm == 23),
                           reads=[(blt, lkey), (brt_, None)], writes=[(bpr, q4)], inc=(nmm == 23), sgc=True)
                        nmm += 1
                fw.op(ACT, lambda: nc.scalar.mul(acc[:, t, :], hb_, ALPHA), reads=[(bhb, None)], writes=[(bacc, t)])

            def phD(g):
                pr_, bpr = prs[g]
                R = [(brt, None)]
                o = [0]

                def al(n):
                    a_ = o[0]
                    o[0] += n
                    return rt[:, a_:a_ + n]
                lg = al(80).rearrange("p (t c) -> p t c", t=4)
                fw.op(DVE, lambda: nc.vector.tensor_tensor(out=lg, in0=pr_[:, 0:80].rearrange("p (t c) -> p t c", t=4), in1=brb[:].unsqueeze(1).broadcast_to([128, 4, 20]), op=ALU.add),
                      reads=[(bpr, None), (bbrb, None)], writes=R)
                gmax = al(4)
                fw.op(DVE, lambda: nc.vector.reduce_max(out=gmax, in_=lg[:, :, 0:4], axis=AX.X), reads=R, writes=R)
                dg = al(16).rearrange("p (t c) -> p t c", t=4)
                fw.op(DVE, lambda: nc.vector.tensor_tensor(out=dg, in0=lg[:, :, 0:4], in1=gmax.unsqueeze(2).broadcast_to([128, 4, 4]), op=ALU.subtract), reads=R, writes=R)
                eg = al(16).rearrange("p (t c) -> p t c", t=4)
                fw.op(ACT, lambda: nc.scalar.activation(out=eg, in_=dg, func=AF.Exp), reads=R, writes=R)
                sumg = al(4)
                fw.op(DVE, lambda: nc.vector.reduce_sum(out=sumg, in_=eg, axis=AX.X), reads=R, writes=R)
                ptop = al(4)
                fw.op(DVE, lambda: nc.vector.reciprocal(ptop, sumg), reads=R, writes=R)
                pen = al(16).rearrange("p (t c) -> p t c", t=4)
                fw.op(DVE, lambda: nc.vector.tensor_scalar(out=pen, in0=dg, scalar1=0.0, scalar2=None, op0=ALU.is_equal), reads=R, writes=R)
                fw.op(DVE, lambda: nc.vector.tensor_scalar(out=pen, in0=pen, scalar1=-1.0, scalar2=1e30, op0=ALU.add, op1=ALU.mult), reads=R, writes=R)
                elm = al(64).rearrange("p (t c) -> p t c", t=4)
                fw.op(DVE, lambda: nc.vector.tensor_tensor(out=elm.rearrange("p t (g e) -> p t g e", g=4), in0=lg[:, :, 4:20].rearrange("p t (g e) -> p t g e", g=4),
                                                           in1=pen.unsqueeze(3).broadcast_to([128, 4, 4, 4]), op=ALU.add), reads=R, writes=R)
                top8 = al(32).rearrange("p (t c) -> p t c", t=4)
                for q4 in range(4):
                    fw.op(DVE, lambda: nc.vector.max(out=top8[:, q4, :], in_=elm[:, q4, :]), reads=R, writes=R)
                d21 = al(4)
                fw.op(DVE, lambda: nc.vector.tensor_tensor(out=d21, in0=top8[:, :, 1], in1=top8[:, :, 0], op=ALU.subtract), reads=R, writes=R)
                e2 = al(4)
                fw.op(ACT, lambda: nc.scalar.activation(out=e2, in_=d21, func=AF.Exp), reads=R, writes=R)
                w1 = al(4)
                fw.op(DVE, lambda: nc.vector.tensor_scalar(out=w1, in0=e2, scalar1=1.0, scalar2=None, op0=ALU.add), reads=R, writes=R)
                fw.op(DVE, lambda: nc.vector.reciprocal(w1, w1), reads=R, writes=R)
                w1p = al(4)
                fw.op(DVE, lambda: nc.vector.tensor_tensor(out=w1p, in0=w1, in1=ptop, op=ALU.mult), reads=R, writes=R)
                w2p = al(4)
                fw.op(DVE, lambda: nc.vector.tensor_tensor(out=w2p, in0=w1p, in1=e2, op=ALU.mult), reads=R, writes=R)
                c1 = al(64).rearrange("p (t c) -> p t c", t=4)
                c2 = al(64).rearrange("p (t c) -> p t c", t=4)
                fw.op(DVE, lambda: nc.vector.tensor_tensor(out=c1, in0=elm, in1=top8[:, :, 0:1].broadcast_to([128, 4, 16]), op=ALU.is_equal), reads=R, writes=R)
                fw.op(DVE, lambda: nc.vector.tensor_tensor(out=c1, in0=c1, in1=w1p.unsqueeze(2).broadcast_to([128, 4, 16]), op=ALU.mult), reads=R, writes=R)
                fw.op(DVE, lambda: nc.vector.tensor_tensor(out=c2, in0=elm, in1=top8[:, :, 1:2].broadcast_to([128, 4, 16]), op=ALU.is_equal), reads=R, writes=R)
                fw.op(DVE, lambda: nc.vector.tensor_tensor(out=c2, in0=c2, in1=w2p.unsqueeze(2).broadcast_to([128, 4, 16]), op=ALU.mult), reads=R, writes=R)
                fw.op(DVE, lambda: nc.vector.tensor_tensor(out=comb[:, g * 4:(g + 1) * 4, :], in0=c1, in1=c2, op=ALU.add), reads=R, writes=[(bcomb, g)])

            for step in range(NT + 3):
                if step >= 3:
                    phC(step - 3)
                    if (step - 3) % 4 == 3 and S5LVL >= 5:
                        phD((step - 3) // 4)
                if 2 <= step <= NT + 1:
                    phB2(step - 2)
                if 1 <= step <= NT:
                    phB1(step - 1)
                if step < NT:
                    phA(step)
            return acc, bacc

        def moe(b, acc, bacc):
            ln2_tile = ln2_setup(b, acc, bacc)
            base = 64 * KB
            hT = [carve(arena, base + i * 4 * KB, [128, 4, 512], BF16, "hT%d" % i) for i in range(2)]
            sab = [carve(arena, base + 8 * KB + i * KB, [128, 512], BF16, "sa%d" % i) for i in range(2)]
            extra = []
            for i in range(3):
                ap_, b_ = carve(arena, base + 10 * KB + i * 8 * KB, [128, 4096], BF16, "wx%d" % i)
                b_.counter = xslot_counters[i]
                extra.append((ap_, b_))
            assert not wq["fifo"]
            saved = (wq["slots"], wq["next"])
            wq["slots"] = list(wslots) + extra
            wq["next"] = 0

            def jobs_for(e):
                return [("w1_%d_%d" % (b, e), [(0, w1_d[e], 8, 512)]), ("w3_%d_%d" % (b, e), [(0, w3_d[e], 8, 512)]), ("w2_%d_%d" % (b, e), [(0, w2_d[e], 4, 1024)])]

            for j in jobs_for(0):
                w_hint(*j)
            pending = []
            for e in range(16):
                ws = [w_get(*j) for j in jobs_for(e)]
                (w1t, bw1), (w3t, bw3), (w2t, bw2) = ws
                for tg in range(4):
                    hT_, bhT = hT[tg % 2]
                    for f in range(4):
                        pa, bpa = proj_fm(pbank(ALL_BANKS), w1t, bw1, f * 128, 128, xT, bxT, tg, tg * 512, 512, 512)
                        pb_, bpb = proj_fm(pbank(ALL_BANKS), w3t, bw3, f * 128, 128, xT, bxT, tg, tg * 512, 512, 512)
                        sa_, bsa = sab[f % 2]
                        fw.op(ACT, lambda: nc.scalar.activation(out=sa_[:], in_=pa[:, :], func=AF.Silu), reads=[(bpa, None)], writes=[(bsa, None)])
                        fw.op(DVE, lambda: nc.vector.tensor_tensor(out=hT_[:, f, :], in0=sa_[:], in1=pb_[:, :], op=ALU.mult),
                              reads=[(bsa, None), (bpb, None)], writes=[(bhT, f)])
                    while pending:
                        pending.pop(0)()
                    if tg == 0 and e + 1 < 16:
                        for j in jobs_for(e + 1):
                            w_hint(*j)

                    def outp(e=e, tg=tg, hT_=hT_, bhT=bhT, w2t=w2t, bw2=bw2):
                        for tt in range(4):
                            t = tg * 4 + tt
                            for hf in range(2):
                                po, bpo = psb(pbank(ALL_BANKS))
                                for f in range(4):
                                    mm(po[:, :], hT_[:, f, tt * 128:(tt + 1) * 128], w2t[:, f * 1024 + hf * 512:f * 1024 + (hf + 1) * 512],
                                       start=(f == 0), stop=(f == 3), reads=[(bhT, None), (bw2, None)], writes=[(bpo, None)])
                                dst = acc[:, t, hf * 512:(hf + 1) * 512]
                                fw.op(DVE, lambda: nc.vector.scalar_tensor_tensor(out=dst, in0=po[:, :], scalar=comb[:, t, e:e + 1], in1=dst, op0=ALU.mult, op1=ALU.add),
                                      reads=[(bpo, None), (bcomb, t // 4), (bacc, t)], writes=[(bacc, t)])
                            if e == 15:
                                ln2_tile(t)
                    pending.append(outp)
            while pending:
                pending.pop(0)()
            assert not wq["fifo"]
            wq["slots"], wq["next"] = saved

        def ln2_setup(b, acc, bacc):
            ot = [carve(tarena, i * 4 * KB, [128, D], F32, "ot%d" % i) for i in range(2)]
            bns, bbns = carve(tarena, 16 * KB, [128, 2, 8], F32, "bns6")
            mv, bmv = carve(tarena, 16 * KB + 64, [128, 4], F32, "mv6")
            fw.dma(SP, lnb[:, 0, :], bc_d[:, 2 * D:3 * D].partition_broadcast(128), writes=[(blnb, None)], counter=blnb.counter)
            fw.dma(SP, lnb[:, 1, :], bc_d[:, 3 * D:4 * D].partition_broadcast(128), writes=[(blnb, None)], counter=blnb.counter)

            def ln2_tile(t):
                ot_, bot = ot[t % 2]
                layer_norm(acc[:, t, :], bacc, t, bns, bbns, mv, bmv, ot_, bot, None)
                fw.dma(SP, out_d[b, t * 128:(t + 1) * 128, :], ot_, reads=[(bot, None)], counter=out_counters[t % 2])
            return ln2_tile

        augcs = [fw.counter("augc%d" % i, 16) for i in range(4)]
        xslot_counters = [fw.counter("xslotc%d" % i, 16) for i in range(3)]

        dumps = []
        for b in range(nb):
            load_T(lambda t: x_d[b, t * 128:(t + 1) * 128, :], NT, xT, bxT, lambda t: t // 4)
            yT, byT = carve(arena, RA1, [128, 8, S], BF16, "yT")
            mg, bmg = carve(arena, RA2, [128, 8, S], BF16, "mg")
            if stop_after == "s5only":
                fw.op(DVE, lambda: nc.vector.memset(mg[:], 0.25), writes=[(bmg, None)])
                acc, bacc = stage5(b, mg, bmg)
                break
            def fold_first(n):
                g0 = 7168 + n * 1024
                return [("fold%d_%d_%d" % (b, n, 0), [(0, w_br_d[n][:, 0:128], 8, 128), (1024, w_in_d[:, g0:g0 + 128], 8, 128)])]
            nxt[0] = fold_first(0) if stop_after != "da" else None
            stage_da(b, yT, byT)
            if stop_after == "da" and b == nb - 1:
                dumps.append((yT, byT))
                break
            nxt[0] = [("rqk%d_%d" % (b, 0), [(0, w_in_d[:, 3072:3072 + 256], 8, 256), (2048, w_in_d[:, 3584:3584 + 256], 8, 256)])]
            fold(b, 0, yT, byT, mg, bmg, True)
            yT, byT = carve(arena, RA1, [128, 8, S], BF16, "yT")
            nxt[0] = fold_first(1) if stop_after != "ret" else None
            stage_ret(b, yT, byT)
            if stop_after == "ret" and b == nb - 1:
                dumps.append((yT, byT))
                break
            nxt[0] = [("mk%d_%d" % (b, 0), [(0, w_kv_d[:, 0:512], 8, 512)])]
            fold(b, 1, yT, byT, mg, bmg, False)
            yT, byT = carve(arena, RA1, [128, 8, S], BF16, "yT")
            nxt[0] = fold_first(2) if stop_after != "mem" else None
            stage_mem(b, yT, byT)
            if stop_after == "mem" and b == nb - 1:
                dumps.append((yT, byT))
                break
            nxt[0] = [("wo%d_0" % b, [(0, w_o_d[:, 0:512], 8, 512)]), ("wo%d_1" % b, [(0, w_o_d[:, 512:1024], 8, 512)])] if stop_after != "fold" else None
            fold(b, 2, yT, byT, mg, bmg, False)
            if stop_after == "fold" and b == nb - 1:
                dumps.append((mg, bmg))
                break
            acc, bacc = stage5(b, mg, bmg)
            if stop_after == "s5" and b == nb - 1:
                break
            moe(b, acc, bacc)
        run_deferred()
        if dbg and dumps:
            t_, b_ = dumps[0]
            dc_ = fw.counter("dbgc", 16)
            for c in range(8):
                fw.dma(POOL, dbg_d[:, c * S:(c + 1) * S], t_[:, c, :], reads=[(b_, None)], counter=dc_)
            SP.eng.wait_ge(dc_.sem, dc_.n)
        if dbg and stop_after in ("s5", "s5only"):
            dc_ = fw.counter("dbgc", 16)
            for t in range(NT):
                fw.dma(SP, out_d[0, t * 128:(t + 1) * 128, :], acc[:, t, :], reads=[(bacc, None)], counter=dc_)
            fw.dma(SP, dbg_d[:, 0:256], comb[:].rearrange("p a b -> p (a b)"), reads=[(bcomb, None)], counter=dc_)
            SP.eng.wait_ge(dc_.sem, dc_.n)
        for c in out_counters:
            if c.n > 0:
                SP.eng.wait_ge(c.sem, c.n)
        stats = {e.name: (e.nins, e.nwait) for e in (PE, ACT, DVE, POOL, SP)}
        print("kernel build: sems", fw.nsem, "ins/waits", stats)
    return nc


def _consts():
    cst = np.zeros((128, C_N), np.float32)
    cst[:, C_ID:C_ID + 128] = np.eye(128, dtype=np.float32)
    kl = np.arange(128)[:, None]
    ql = np.arange(128)[None, :]
    cst[:, C_MASK:C_MASK + 128] = np.where(kl > ql, MASKNEG, 0.0)
    for h in range(4):
        g = GAMMA[h]
        cst[:, C_MRET + h * 128:C_MRET + (h + 1) * 128] = np.where(ql >= kl, np.power(g, -(kl + 1.0)), 0.0)
        cst[:, C_DQ + h * 128:C_DQ + (h + 1) * 128] = np.power(g, ql + 1.0) * np.ones((128, 1))
        cst[:, C_KDEC + h] = np.power(g, 127.0 - np.arange(128)) * (128.0 ** -0.5)
        cst[:, C_KD2 + h] = np.power(g, 127.0 - np.arange(128))
    cst[:, C_NH] = -0.5
    aug = np.zeros((2, 4, S), np.float32)
    t = np.arange(S)
    aug[0, 0] = -(t // 128) * 128.0
    aug[0, 1] = -(t % 128)
    aug[0, 2] = 1.0
    aug[0, 3] = 1.0
    aug[1, 0] = 1.0
    aug[1, 1] = 1.0
    aug[1, 2] = (t // 128) * 128.0
    aug[1, 3] = (t % 128)
    return cst, aug


_CACHE = {}


def _host_inputs(inputs, nb, cores):
    f = lambda a: np.ascontiguousarray(np.asarray(a, dtype=np.float32))
    cst, aug = _consts()
    pv = np.zeros((128, 41), np.float32)
    pv[:, 0:24] = f(inputs["b_gate"])[0].reshape(24, 128).T
    pv[:, 24:32] = f(inputs["ret_gn_g"])[0].reshape(8, 128).T
    pv[:, 32:40] = f(inputs["ret_gn_b"])[0].reshape(8, 128).T
    pv[:, 40] = f(inputs["da_norm_g"])[0]
    wrc = np.concatenate([f(inputs["w_rg"])[0], f(inputs["w_re"])[0]], axis=1)
    wr = np.ascontiguousarray(wrc.reshape(8, 128, 20).transpose(1, 0, 2).reshape(128, 160))
    bc = np.concatenate([f(inputs["ln1_g"])[0], f(inputs["ln1_b"])[0], f(inputs["ln2_g"])[0], f(inputs["ln2_b"])[0],
                         f(inputs["b_rg"])[0], f(inputs["b_re"])[0], f(inputs["da_lambda"])[0].reshape(-1)])[None, :]
    shared = {
        "w_in": f(inputs["w_in"])[0], "w_mem_kv": f(inputs["w_mem_kv"])[0], "w_branch": f(inputs["w_branch"])[0],
        "w_o": f(inputs["w_o"])[0], "w1": f(inputs["w1"])[0], "w3": f(inputs["w3"])[0], "w2": f(inputs["w2"])[0],
        "cst": cst, "aug": aug, "pv": pv, "wr": wr, "bc": np.ascontiguousarray(bc),
    }
    x = f(inputs["x"])
    mem = f(inputs["mem"])
    maps = []
    for c in cores:
        m = dict(shared)
        m["x"] = np.ascontiguousarray(x[c * nb:(c + 1) * nb])
        m["mem"] = np.ascontiguousarray(mem[c * nb:(c + 1) * nb])
        maps.append(m)
    return maps


def kernel(**inputs):
    nb = 2
    ncores = 8
    if "nc" not in _CACHE:
        _CACHE["nc"] = build(nb=nb)
    nc = _CACHE["nc"]
    maps = _host_inputs(inputs, nb, list(range(ncores)))
    res = run_bass_kernel_spmd(nc, maps, core_ids=list(range(ncores)))
    out = np.concatenate([r["out"] for r in res.results], axis=0)
    return out.astype(np.float32)
```

```python
import contextlib
import math
import numpy as np
import concourse.bass as bass
import concourse.mybir as mybir
from concourse.bass_utils import run_bass_kernel_spmd

F32 = mybir.dt.float32
BF16 = mybir.dt.bfloat16
AF = mybir.ActivationFunctionType
ALU = mybir.AluOpType
AX = mybir.AxisListType

S5LVL = 9
S = 2048
D = 1024
NT = 16
ALPHA = 2.0 ** 0.25
EPS = 1e-5
LAMBDA_INIT = 0.8 - 0.6 * math.exp(-0.3 * 0)
GAMMA = [1.0 - 2.0 ** (-5.0 - h) for h in range(4)]
SLOPE = [2.0 ** (-(h + 1)) for h in range(8)]
MASKNEG = -float(2 ** 20)

C_ID, C_MASK, C_MRET, C_DQ, C_KDEC, C_NH, C_KD2, C_N = 0, 128, 256, 768, 1280, 1284, 1288, 1292


class Counter:
    def __init__(self, sem, step):
        self.sem = sem
        self.step = step
        self.n = 0


class Buf:
    def __init__(self, name, counter=None):
        self.name = name
        self.counter = counter
        self.w = {}
        self.r = {}

    def _keys(self, key):
        if key is None:
            return set(self.w) | set(self.r) | {None}
        return {key, None}

    def raw(self, key):
        return [self.w[k] for k in self._keys(key) if k in self.w]

    def war(self, key):
        out = []
        for k in self._keys(key):
            out += self.r.get(k, [])
        return out

    def add_read(self, key, tok):
        lst = self.r.setdefault(key, [])
        lst.append(tok)
        if len(lst) > 24:
            best = {}
            for c, v in lst:
                if best.get(c, 0) < v:
                    best[c] = v
            self.r[key] = list(best.items())

    def set_write(self, key, tok):
        if key is None:
            self.w = {None: tok}
            self.r = {}
        else:
            self.w[key] = tok
            self.r[key] = []

    def all_tokens(self):
        out = list(self.w.values())
        for v in self.r.values():
            out += v
        return out

    def absorb(self, other):
        best = {}
        for (c, v) in other.all_tokens():
            if best.get(c, 0) < v:
                best[c] = v
        lst = self.r.setdefault(None, [])
        for c, v in best.items():
            lst.append((c, v))


class Eng:
    def __init__(self, name, eng, counter, is_pe=False):
        self.name = name
        self.eng = eng
        self.counter = counter
        self.seen = {}
        self.is_pe = is_pe
        self.nwait = 0
        self.nins = 0


class FW:
    def __init__(self, nc, es):
        self.nc = nc
        self.es = es
        self.nsem = 0
        self.PE = Eng("pe", nc.tensor, self.counter("pe", 1), is_pe=True)
        self.ACT = Eng("act", nc.scalar, self.counter("act", 1))
        self.DVE = Eng("dve", nc.vector, self.counter("dve", 1))
        self.POOL = Eng("pool", nc.gpsimd, self.counter("pool", 1))
        self.SP = Eng("sp", nc.sync, self.counter("sp", 1))

    def counter(self, name, step):
        sem = self.es.enter_context(self.nc.semaphore("s_" + name))
        self.nsem += 1
        return Counter(sem, step)

    def sbuf(self, name, shape, dt, dma=False):
        t = self.es.enter_context(self.nc.sbuf_tensor("sb_" + name, shape, dt))
        b = Buf(name, self.counter("d_" + name, 16) if dma else None)
        return t, b

    def psum(self, name, shape, dt):
        t = self.es.enter_context(self.nc.psum_tensor("pp_" + name, shape, dt))
        return t, Buf(name)

    def _wait(self, E, deps):
        need = {}
        for (c, v) in deps:
            if need.get(c, 0) < v:
                need[c] = v
        for c, v in need.items():
            if E.seen.get(c, 0) >= v:
                continue
            E.eng.wait_ge(c.sem, v)
            E.seen[c] = v
            E.nwait += 1

    def op(self, E, fn, reads=(), writes=(), inc=True):
        deps = []
        own = E.counter
        for b, k in reads:
            deps += b.raw(k)
        if not E.is_pe:
            for b, k in writes:
                deps += [t for t in (b.raw(k) + b.war(k)) if t[0] is not own]
        else:
            for b, k in writes:
                deps += b.raw(k) + b.war(k)
            deps = [d for d in deps if d[0] is not own]
        self._wait(E, deps)
        ins = fn()
        E.nins += 1
        if inc:
            own.n += 1
            ins.then_inc(own.sem, 1)
            tok = (own, own.n)
        else:
            tok = (own, own.n + 1)
        for b, k in reads:
            b.add_read(k, tok)
        for b, k in writes:
            b.set_write(k, tok)
        return ins

    def dma(self, E, out, in_, reads=(), writes=(), counter=None, **kw):
        deps = []
        for b, k in reads:
            deps += b.raw(k)
        for b, k in writes:
            deps += b.raw(k) + b.war(k)
        self._wait(E, deps)
        ins = E.eng.dma_start(out=out, in_=in_, **kw)
        counter.n += 16
        ins.then_inc(counter.sem, 16)
        tok = (counter, counter.n)
        for b, k in reads:
            b.add_read(k, tok)
        for b, k in writes:
            b.set_write(k, tok)
        return ins


def build(nb=2, stop_after=None, dbg=False):
    nc = bass.Bass("TRN2", target_bir_lowering=False)

    def din(name, shape):
        return nc.dram_tensor(name, shape, F32, kind="ExternalInput").ap()

    x_d = din("x", [nb, S, D])
    mem_d = din("mem", [nb, 256, D])
    w_in_d = din("w_in", [D, 10240])
    w_kv_d = din("w_mem_kv", [D, 2048])
    w_br_d = din("w_branch", [3, D, D])
    w_o_d = din("w_o", [D, D])
    w1_d = din("w1", [16, D, 512])
    w3_d = din("w3", [16, D, 512])
    w2_d = din("w2", [16, 512, D])
    cst_d = din("cst", [128, C_N])
    aug_d = din("aug", [2, 4, S])
    pv_d = din("pv", [128, 41])
    wr_d = din("wr", [128, 160])
    bc_d = din("bc", [1, 4 * D + 20 + 256])
    out_d = nc.dram_tensor("out", [nb, S, D], F32, kind="ExternalOutput").ap()
    if dbg:
        dbg_d = nc.dram_tensor("dbg", [128, 8 * S], F32, kind="ExternalOutput").ap()

    es = contextlib.ExitStack()
    with es:
        fw = FW(nc, es)
        PE, ACT, DVE, POOL, SP = fw.PE, fw.ACT, fw.DVE, fw.POOL, fw.SP

        cst, bcst = fw.sbuf("cst", [128, C_N], F32, dma=True)
        idb, bidb = fw.sbuf("idb", [128, 128], BF16)
        maskb, bmaskb = fw.sbuf("maskb", [128, 128], BF16)
        pv, bpv = fw.sbuf("pv", [128, 48], F32, dma=True)
        wr, bwr = fw.sbuf("wr", [128, 160], F32, dma=True)
        brb, bbrb = fw.sbuf("brb", [128, 20], F32, dma=True)
        lvb, blvb = fw.sbuf("lvb", [128, 256], F32, dma=True)
        lamt, blamt = fw.sbuf("lamt", [128, 16], F32)
        lnb, blnb = fw.sbuf("lnb", [128, 2, D], F32, dma=True)
        xT, bxT = fw.sbuf("xT", [128, 8, S], BF16)
        comb, bcomb = fw.sbuf("comb", [128, NT, 16], F32)
        NSLOT = 3
        wslots = []
        for i in range(NSLOT):
            wslots.append(fw.sbuf("wslot%d" % i, [128, 4096], BF16, dma=True))
        PT = [fw.sbuf("pt%d" % i, [128, 512], BF16) for i in range(4)]
        xst = [fw.sbuf("xst%d" % i, [128, D], BF16, dma=True) for i in range(2)]
        ARENA_B = 104 * 1024
        arena, _ = fw.sbuf("arena", [128, ARENA_B // 2], BF16)
        TA_B = 19 * 1024
        tarena, _ = fw.sbuf("tarena", [128, TA_B // 2], BF16)
        PS = [fw.psum("ps%d" % i, [128, 512], F32) for i in range(8)]

        out_counters = [fw.counter("outc%d" % i, 16) for i in range(2)]

        live = {id(arena): [], id(tarena): []}

        def carve(ar, off, shape, dt, name):
            esz = 4 if dt == F32 else 2
            n = 1
            for s_ in shape[1:]:
                n *= s_
            nbytes = n * esz
            assert off % 4 == 0
            lim = ARENA_B if ar is arena else TA_B
            assert off + nbytes <= lim, (name, off, nbytes, lim)
            ap = ar[:, off // 2:(off + nbytes) // 2]
            if dt == F32:
                ap = ap.bitcast(F32)
            if len(shape) == 3:
                ap = ap.rearrange("p (a b) -> p a b", a=shape[1])
            elif len(shape) == 4:
                ap = ap.rearrange("p (a b c) -> p a b c", a=shape[1], b=shape[2])
            b = Buf(name)
            lst = live[id(ar)]
            keep = []
            for (lo, hi, ob) in lst:
                if lo < off + nbytes and off < hi:
                    b.absorb(ob)
                    if lo < off or hi > off + nbytes:
                        keep.append((lo, hi, ob))
                else:
                    keep.append((lo, hi, ob))
            keep.append((off, off + nbytes, b))
            live[id(ar)] = keep
            return ap, b

        KB = 1024
        RB0, RA1, RA2 = 0, 40 * KB, 72 * KB

        bank_rr = [0]

        def pbank(lst):
            b = lst[bank_rr[0] % len(lst)]
            bank_rr[0] += 1
            return b

        def copy(E, out, in_, reads, writes, scale=None):
            if E is ACT:
                if scale is None:
                    return fw.op(ACT, lambda: nc.scalar.copy(out, in_), reads=reads, writes=writes)
                return fw.op(ACT, lambda: nc.scalar.activation(out=out, in_=in_, func=AF.Copy, scale=scale), reads=reads, writes=writes)
            if scale is None:
                return fw.op(DVE, lambda: nc.vector.tensor_copy(out, in_), reads=reads, writes=writes)
            return fw.op(DVE, lambda: nc.vector.tensor_scalar(out=out, in0=in_, scalar1=scale, scalar2=None, op0=ALU.mult), reads=reads, writes=writes)

        alt = [0]

        def alt_eng():
            alt[0] += 1
            return ACT if alt[0] % 2 else DVE

        def mm(out, lhsT, rhs, start, stop, reads, writes, inc=None, sgc=False):
            return fw.op(PE, lambda: nc.tensor.matmul(out, lhsT=lhsT, rhs=rhs, start=start, stop=stop, skip_group_check=sgc),
                         reads=reads, writes=writes, inc=(stop if inc is None else inc))

        def tr(out, in_, ident, reads, writes, inc):
            return fw.op(PE, lambda: nc.tensor.transpose(out, in_, ident), reads=reads, writes=writes, inc=inc)

        wq = {"slots": list(wslots), "next": 0, "fifo": []}

        def w_load(pieces):
            sl = wq["slots"][wq["next"] % len(wq["slots"])]
            wq["next"] += 1
            t, b = sl
            for (off, src, k, n) in pieces:
                dst = t[:, off:off + k * n].rearrange("p (k n) -> p k n", k=k)
                fw.dma(POOL, dst, src.rearrange("(k p) n -> p k n", p=128), writes=[(b, None)], counter=b.counter)
            return sl

        def w_hint(key, pieces):
            wq["fifo"].append((key, w_load(pieces)))

        def w_get(key, pieces):
            if wq["fifo"] and wq["fifo"][0][0] == key:
                return wq["fifo"].pop(0)[1]
            assert not wq["fifo"], (key, wq["fifo"][0][0])
            return w_load(pieces)

        nxt = [None]

        def fire_next():
            if nxt[0] is not None:
                for j in nxt[0]:
                    w_hint(*j)
                nxt[0] = None

        def run_jobs(jobs, fn, fire=True):
            for i, (key, pieces) in enumerate(jobs):
                w = w_get(key, pieces)
                if i + 1 < len(jobs):
                    w_hint(*jobs[i + 1])
                elif fire:
                    fire_next()
                fn(i, w)

        sm_off = [0]

        def smalloc(n, name):
            o = sm_off[0]
            sm_off[0] += n
            assert sm_off[0] <= 256
            return sm[:, o:o + n], Buf(name)

        fw.dma(SP, cst[:], cst_d[:, :], writes=[(bcst, None)], counter=bcst.counter)
        fw.dma(SP, pv[:, 0:41], pv_d[:, :], writes=[(bpv, None)], counter=bpv.counter)
        fw.dma(SP, wr[:], wr_d[:, :], writes=[(bwr, None)], counter=bwr.counter)
        fw.dma(SP, brb[:], bc_d[:, 4 * D:4 * D + 20].partition_broadcast(128), writes=[(bbrb, None)], counter=bbrb.counter)
        fw.dma(SP, lvb[:], bc_d[:, 4 * D + 20:4 * D + 276].partition_broadcast(128), writes=[(blvb, None)], counter=blvb.counter)
        fw.op(DVE, lambda: nc.vector.tensor_copy(idb[:], cst[:, C_ID:C_ID + 128]), reads=[(bcst, None)], writes=[(bidb, None)])
        fw.op(DVE, lambda: nc.vector.tensor_copy(maskb[:], cst[:, C_MASK:C_MASK + 128]), reads=[(bcst, None)], writes=[(bmaskb, None)])
        wrh, bwrh = fw.sbuf("wrh", [128, 160], BF16)
        wrl, bwrl = fw.sbuf("wrl", [128, 160], BF16)
        fw.op(DVE, lambda: nc.vector.tensor_copy(wrh[:], wr[:]), reads=[(bwr, None)], writes=[(bwrh, None)])
        fw.op(DVE, lambda: nc.vector.tensor_tensor(out=wrl[:], in0=wr[:], in1=wrh[:], op=ALU.subtract), reads=[(bwr, None), (bwrh, None)], writes=[(bwrl, None)])
        idf = cst[:, C_ID:C_ID + 128]
        nhalf = cst[:, C_NH:C_NH + 1]
        fw.op(DVE, lambda: nc.vector.tensor_scalar(out=pv[:, 0:40], in0=pv[:, 0:40], scalar1=0.5, scalar2=None, op0=ALU.mult), reads=[(bpv, None)], writes=[(bpv, None)])
        fw.op(DVE, lambda: nc.vector.tensor_scalar(out=pv[:, 40:41], in0=pv[:, 40:41], scalar1=(1.0 - LAMBDA_INIT), scalar2=None, op0=ALU.mult), reads=[(bpv, None)], writes=[(bpv, None)])
        fw.op(DVE, lambda: nc.vector.memset(lamt[:], 0.0), writes=[(blamt, None)])
        lv4 = lvb[:].rearrange("p (a b) -> p a b", a=4)
        lprod, blprod = fw.sbuf("lprod", [128, 2, 64], F32)
        fw.op(DVE, lambda: nc.vector.tensor_tensor(out=lprod[:, 0, :], in0=lv4[:, 0, :], in1=lv4[:, 1, :], op=ALU.mult), reads=[(blvb, None)], writes=[(blprod, None)])
        fw.op(DVE, lambda: nc.vector.tensor_tensor(out=lprod[:, 1, :], in0=lv4[:, 2, :], in1=lv4[:, 3, :], op=ALU.mult), reads=[(blvb, None)], writes=[(blprod, None)])
        fw.op(DVE, lambda: nc.vector.reduce_sum(out=lamt[:, 0:2], in_=lprod[:], axis=AX.X), reads=[(blprod, None)], writes=[(blamt, None)])
        fw.op(ACT, lambda: nc.scalar.activation(out=lamt[:, 2:4], in_=lamt[:, 0:2], func=AF.Exp), reads=[(blamt, None)], writes=[(blamt, None)])
        fw.op(DVE, lambda: nc.vector.tensor_tensor(out=lamt[:, 4:5], in0=lamt[:, 2:3], in1=lamt[:, 3:4], op=ALU.subtract), reads=[(blamt, None)], writes=[(blamt, None)])
        fw.op(DVE, lambda: nc.vector.tensor_scalar(out=lamt[:, 5:6], in0=lamt[:, 4:5], scalar1=LAMBDA_INIT, scalar2=None, op0=ALU.add), reads=[(blamt, None)], writes=[(blamt, None)])
        lam = lamt[:, 5:6]

        PROJ_BANKS = [6, 7]
        ALL_BANKS = list(range(8))

        def psb(i):
            return PS[i][0], PS[i][1]

        def load_T(src_rows, ntile, dstT, bdst, keyfn):
            if ntile > 2:
                stg = [(xst[i][0][:], xst[i][1]) for i in range(2)] + [(wslots[i][0][:, 0:D], wslots[i][1]) for i in range(NSLOT)]
            else:
                stg = [(xst[i][0][:], xst[i][1]) for i in range(2)]
            LA = len(stg) - 1

            def issue(t):
                st, bst = stg[t % len(stg)]
                fw.dma(POOL, st, src_rows(t), writes=[(bst, None)], counter=bst.counter)
            for t in range(min(LA, ntile)):
                issue(t)
            for t in range(ntile):
                if t + LA < ntile:
                    issue(t + LA)
                st, bst = stg[t % len(stg)]
                bk = pbank(PROJ_BANKS)
                pt_, bpt = psb(bk)
                pb = pt_[:].bitcast(BF16)
                for c in range(8):
                    tr(pb[:, c * 128:(c + 1) * 128], st[:, c * 128:(c + 1) * 128], idb[:],
                       reads=[(bst, None), (bidb, None)], writes=[(bpt, None)], inc=(c == 7))
                copy(alt_eng(), dstT[:, :, t * 128:(t + 1) * 128], pb.rearrange("p (c t) -> p c t", c=8),
                     reads=[(bpt, None)], writes=[(bdst, keyfn(t))])

        def proj_fm(bank, wt, bw, col0, ncol, rhsT, brhs, rkey, tok0, ntok, kstride):
            pt_, bpt = psb(bank)
            for k in range(8):
                mm(pt_[0:ncol, 0:ntok], wt[:, k * kstride + col0:k * kstride + col0 + ncol], rhsT[:, k, tok0:tok0 + ntok],
                   start=(k == 0), stop=(k == 7), reads=[(bw, None), (brhs, rkey)], writes=[(bpt, None)])
            return pt_, bpt

        def proj_tm(bank, wt, bw, col0, ncol, lhsTt, blhs, lkey, tok0, kstride):
            pt_, bpt = psb(bank)
            for k in range(8):
                mm(pt_[:, 0:ncol], lhsTt[:, k, tok0:tok0 + 128], wt[:, k * kstride + col0:k * kstride + col0 + ncol],
                   start=(k == 0), stop=(k == 7), reads=[(bw, None), (blhs, lkey)], writes=[(bpt, None)])
            return pt_, bpt

        deferred = []

        def run_deferred():
            while deferred:
                deferred.pop(0)()

        def stage_da(b, yT, byT):
            qA, bqA = carve(arena, RB0 + 0 * KB, [128, S], BF16, "qA")
            qB, bqB = carve(arena, RB0 + 4 * KB, [128, S], BF16, "qB")
            kA, bkA = carve(arena, RB0 + 8 * KB, [128, S], BF16, "kA")
            kB, bkB = carve(arena, RB0 + 12 * KB, [128, S], BF16, "kB")
            V1, bV1 = carve(arena, RB0 + 16 * KB, [128, NT, 4, 130], BF16, "V1")
            aa, baa = carve(tarena, 0, [128, 4, 128], F32, "aa")
            t1, bt1 = carve(tarena, 11 * KB, [128, 4, 128], F32, "t1")
            yt4 = [carve(tarena, 3 * KB + i * KB, [128, 4, 128], BF16, "yt4_%d" % i) for i in range(2)]
            rr, brr = carve(tarena, 5 * KB, [128, 16], F32, "rr")
            ss, bss = carve(tarena, 5 * KB + 64, [128, 8], F32, "ss")
            accs, baccs = carve(tarena, 6 * KB, [128, 1164], F32, "accs")
            for (t_, b_, lo, hi, ar0, qk) in ((qA, bqA, 64, 128, 64, 0), (qB, bqB, 0, 64, 0, 0), (kA, bkA, 64, 128, 64, 1), (kB, bkB, 0, 64, 0, 1)):
                fw.op(DVE, lambda: nc.vector.memset(t_[lo:hi, :], 0.0), writes=[(b_, None)])
                fw.dma(POOL, t_[ar0:ar0 + 4, :], aug_d[qk, :, :], writes=[(b_, None)], counter=augcs[(ar0 // 64) * 2 + qk])
            fw.op(DVE, lambda: nc.vector.memset(V1[:, :, :, 128:129], 1.0), writes=[(bV1, "ones")])
            ST_BANKS = [0, 1, 2]

            def acc_ap(c, ii):
                s_ = c * 4 + ii
                bk = 3 + s_ // 3
                col = (s_ % 3) * 129
                return PS[bk][0][:, col:col + 129], PS[bk][1], s_

            grp = [0]
            for hg in range(2):
                vjob = ("dav%d_%d" % (b, hg), [(0, w_in_d[:, 2048 + hg * 512:2048 + (hg + 1) * 512], 8, 512)])
                wv, bwv = w_get(*vjob)
                qk_jobs = []
                for h in range(4):
                    H = hg * 4 + h
                    qk_jobs.append(("daqk%d_%d" % (b, H), [(0, w_in_d[:, H * 128:(H + 1) * 128], 8, 128),
                                                          (1024, w_in_d[:, 1024 + H * 128:1024 + (H + 1) * 128], 8, 128)]))
                w_hint(*qk_jobs[0])
                for t in range(NT):
                    pt_, bpt = proj_tm(pbank(PROJ_BANKS), wv, bwv, 0, 512, xT, bxT, t // 4, t * 128, 512)
                    copy(alt_eng(), V1[:, t, :, 0:128], pt_[:, :].rearrange("p (h e) -> p h e", h=4),
                         reads=[(bpt, None)], writes=[(bV1, t)])
                for h in range(4):
                    H = hg * 4 + h
                    wqk, bwqk = w_get(*qk_jobs[h])
                    if h + 1 < 4:
                        w_hint(*qk_jobs[h + 1])
                    elif hg == 0:
                        w_hint("dav%d_%d" % (b, 1), [(0, w_in_d[:, 2048 + 512:2048 + 1024], 8, 512)])
                    else:
                        fire_next()
                    qs = 2.0 ** (H - 2)
                    for tg in range(4):
                        pt_, bpt = proj_fm(pbank(PROJ_BANKS), wqk, bwqk, 0, 128, xT, bxT, tg, tg * 512, 512, 128)
                        copy(ACT, qA[0:64, tg * 512:(tg + 1) * 512], pt_[0:64, :], reads=[(bpt, None)], writes=[(bqA, tg)], scale=qs)
                        copy(DVE, qB[64:128, tg * 512:(tg + 1) * 512], pt_[64:128, :], reads=[(bpt, None)], writes=[(bqB, tg)], scale=qs)
                        pt_, bpt = proj_fm(pbank(PROJ_BANKS), wqk, bwqk, 1024, 128, xT, bxT, tg, tg * 512, 512, 128)
                        copy(ACT, kA[0:64, tg * 512:(tg + 1) * 512], pt_[0:64, :], reads=[(bpt, None)], writes=[(bkA, tg)])
                        copy(DVE, kB[64:128, tg * 512:(tg + 1) * 512], pt_[64:128, :], reads=[(bpt, None)], writes=[(bkB, tg)])
                    pend = []

                    def epilogue(I):
                            run_deferred()
                            yt, byt = yt4[(H * 4 + I) % 2]
                            for bk3 in range(3):
                                ncp = 387 if bk3 < 2 else 258
                                fw.op(DVE, lambda: nc.vector.tensor_copy(accs[:, bk3 * 387:bk3 * 387 + ncp], PS[3 + bk3][0][:, 0:ncp]),
                                      reads=[(PS[3 + bk3][1], None)], writes=[(baccs, bk3)])
                            A4 = accs[:, 0:1032].rearrange("p (c i e) -> p c i e", c=2, i=4)
                            RA = [(baccs, None)]
                            fw.op(DVE, lambda: nc.vector.reciprocal(rr[:, 0:8].rearrange("p (c i) -> p c i", c=2), A4[:, :, :, 128]), reads=RA, writes=[(brr, None)])
                            fw.op(DVE, lambda: nc.vector.tensor_scalar(out=rr[:, 8:12], in0=rr[:, 4:8], scalar1=lam, scalar2=None, op0=ALU.mult),
                                  reads=[(brr, None), (blamt, None)], writes=[(brr, None)])
                            fw.op(DVE, lambda: nc.vector.tensor_tensor(out=t1[:], in0=A4[:, 1, :, 0:128], in1=rr[:, 8:12].unsqueeze(2).broadcast_to([128, 4, 128]), op=ALU.mult),
                                  reads=RA + [(brr, None)], writes=[(bt1, None)])
                            fw.op(DVE, lambda: nc.vector.tensor_tensor(out=aa[:], in0=A4[:, 0, :, 0:128], in1=rr[:, 0:4].unsqueeze(2).broadcast_to([128, 4, 128]), op=ALU.mult),
                                  reads=RA + [(brr, None)], writes=[(baa, None)])
                            fw.op(DVE, lambda: nc.vector.tensor_tensor(out=aa[:], in0=aa[:], in1=t1[:], op=ALU.subtract), reads=[(baa, None), (bt1, None)], writes=[(baa, None)])
                            fw.op(DVE, lambda: nc.vector.tensor_tensor(out=t1[:], in0=aa[:], in1=aa[:], op=ALU.mult), reads=[(baa, None)], writes=[(bt1, None)])
                            fw.op(DVE, lambda: nc.vector.reduce_sum(out=ss[:, 0:4], in_=t1[:], axis=AX.X), reads=[(bt1, None)], writes=[(bss, None)])
                            fw.op(POOL, lambda: nc.gpsimd.tensor_scalar(out=ss[:, 4:8], in0=ss[:, 0:4], scalar1=1.0 / 128.0, scalar2=EPS, op0=ALU.mult, op1=ALU.add),
                                  reads=[(bss, None)], writes=[(bss, None)])
                            fw.op(POOL, lambda: nc.gpsimd.tensor_tensor(out=ss[:, 0:4], in0=ss[:, 4:8], in1=nhalf.broadcast_to([128, 4]), op=ALU.pow),
                                  reads=[(bss, None), (bcst, None)], writes=[(bss, None)])
                            fw.op(DVE, lambda: nc.vector.tensor_tensor(out=yt[:], in0=aa[:], in1=ss[:, 0:4].unsqueeze(2).broadcast_to([128, 4, 128]), op=ALU.mult),
                                  reads=[(baa, None), (bss, None)], writes=[(byt, None)])

                            def part2(yt=yt, byt=byt, H=H, I=I):
                                bk = pbank(PROJ_BANKS)
                                pt_, bpt = psb(bk)
                                pb = pt_[:].bitcast(BF16)
                                for ii in range(4):
                                    tr(pb[:, ii * 128:(ii + 1) * 128], yt[:, ii, :], idb[:], reads=[(byt, None), (bidb, None)], writes=[(bpt, None)], inc=(ii == 3))
                                fw.op(DVE, lambda: nc.vector.tensor_scalar(out=yT[:, H, I * 512:(I + 1) * 512], in0=pb[:, 0:512], scalar1=pv[:, 40:41], scalar2=None, op0=ALU.mult),
                                      reads=[(bpt, None), (bpv, None)], writes=[(byT, (H, I))])
                            deferred.append(part2)

                    def do_av(st):
                        (I, c, j, i0, ncols, pslot) = st
                        ptt, bptt = PT[pslot]
                        for i in range(i0, 4 * I + 4):
                            ii = i - 4 * I
                            ap_, bacc, s_ = acc_ap(c, ii)
                            mm(ap_, ptt[:, (i - i0) * 128:(i - i0 + 1) * 128], V1[:, j, h, 0:129],
                               start=(j == 0 and s_ % 3 == 0), stop=(j == i), reads=[(bptt, None), (bV1, None)], writes=[(bacc, s_)], inc=(j == i or i == 4 * I + 3), sgc=True)
                        if c == 1 and j == 4 * I + 3:
                            epilogue(I)

                    for I in range(4):
                        for c in range(2):
                            qX, bqX, kX, bkX = (qA, bqA, kA, bkA) if c == 0 else (qB, bqB, kB, bkB)
                            for j in range(4 * I + 4):
                                i0 = max(4 * I, j)
                                ncols = (4 * I + 4 - i0) * 128
                                diag = (j >= 4 * I)
                                stb = pbank(ST_BANKS)
                                pst, bpst = psb(stb)
                                mm(pst[:, 0:ncols], kX[:, j * 128:(j + 1) * 128], qX[:, i0 * 128:i0 * 128 + ncols],
                                   start=True, stop=(not diag), reads=[(bkX, None), (bqX, None)], writes=[(bpst, None)])
                                if diag:
                                    mm(pst[:, 0:128], idb[:], maskb[:], start=False, stop=True,
                                       reads=[(bidb, None), (bmaskb, None)], writes=[(bpst, None)])
                                pslot = grp[0] % 4
                                grp[0] += 1
                                ptt, bptt = PT[pslot]
                                fw.op(ACT, lambda: nc.scalar.activation(out=ptt[:, 0:ncols], in_=pst[:, 0:ncols], func=AF.Exp, scale=SLOPE[H]),
                                      reads=[(bpst, None)], writes=[(bptt, None)])
                                pend.append((I, c, j, i0, ncols, pslot))
                                if len(pend) > 2:
                                    do_av(pend.pop(0))
                    while pend:
                        do_av(pend.pop(0))
            run_deferred()

        def stage_ret(b, yT, byT):
            st_f, bst_f = carve(tarena, 0, [128, 2, 256], F32, "st_f")
            st_b, bst_b = carve(tarena, 2 * KB, [128, 2, 256], BF16, "st_b")
            stm = [carve(tarena, 3 * KB + i * 256, [128, 128], BF16, "stm%d" % i) for i in range(4)]
            zt, bzt = carve(tarena, 4 * KB, [128, 2, 4, 512], BF16, "zt")
            zt = zt.rearrange("p g q (h e) -> p g q h e", h=2)
            thb = [carve(tarena, 12 * KB + i * KB, [128, 512], BF16, "thb%d" % i) for i in range(2)]
            sgb = [carve(tarena, 14 * KB + i * KB, [128, 512], BF16, "sgb%d" % i) for i in range(2)]
            yzb = [carve(tarena, 16 * KB + i * KB, [128, 512], BF16, "yzb%d" % i) for i in range(2)]
            bns, bbns = carve(tarena, 18 * KB, [128, 2, 8], F32, "bns")
            mv, bmv = carve(tarena, 18 * KB + 64, [128, 4, 4], F32, "mv")
            mv = mv.rearrange("p (a h) c -> p a h c", a=2)
            for hp in range(2):
                qT = [carve(arena, RB0 + i * 4 * KB, [128, S], BF16, "rq%d" % i) for i in range(2)]
                kT = [carve(arena, RB0 + 8 * KB + i * 4 * KB, [128, S], BF16, "rk%d" % i) for i in range(2)]
                kt = [carve(arena, RB0 + 16 * KB + i * 4 * KB, [128, NT, 128], BF16, "rkt%d" % i) for i in range(2)]
                vv, bvv = carve(arena, RB0 + 24 * KB, [128, NT, 2, 256], BF16, "rv")
                c0 = 3072 + hp * 256
                c1 = 3584 + hp * 256
                wqk, bwqk = w_get("rqk%d_%d" % (b, hp), [(0, w_in_d[:, c0:c0 + 256], 8, 256), (2048, w_in_d[:, c1:c1 + 256], 8, 256)])
                w_hint("rv%d_%d" % (b, hp), [(0, w_in_d[:, 4096 + hp * 512:4096 + (hp + 1) * 512], 8, 512)])
                for hh in range(2):
                    H = hp * 2 + hh
                    for tg in range(4):
                        pt_, bpt = proj_fm(pbank(ALL_BANKS), wqk, bwqk, hh * 128, 128, xT, bxT, tg, tg * 512, 512, 256)
                        fw.op(DVE, lambda: nc.vector.tensor_tensor(out=qT[hh][0][:, tg * 512:(tg + 1) * 512].rearrange("p (a q) -> p a q", a=4),
                                                                   in0=pt_[:, :].rearrange("p (a q) -> p a q", a=4),
                                                                   in1=cst[:, C_DQ + H * 128:C_DQ + (H + 1) * 128].unsqueeze(1).broadcast_to([128, 4, 128]),
                                                                   op=ALU.mult),
                              reads=[(bpt, None), (bcst, None)], writes=[(qT[hh][1], tg)])
                        pt_, bpt = proj_fm(pbank(ALL_BANKS), wqk, bwqk, 2048 + hh * 128, 128, xT, bxT, tg, tg * 512, 512, 256)
                        copy(ACT, kT[hh][0][:, tg * 512:(tg + 1) * 512], pt_[:, :], reads=[(bpt, None)], writes=[(kT[hh][1], tg)], scale=128.0 ** -0.5)
                for t4 in range(NT // 4):
                    pt_, bpt = psb(pbank(ALL_BANKS))
                    pbv = pt_[:].bitcast(BF16).rearrange("p (t h d) -> p t h d", t=4, h=2)
                    for q4 in range(4):
                        t = t4 * 4 + q4
                        for hh in range(2):
                            tr(pbv[:, q4, hh, :], kT[hh][0][:, t * 128:(t + 1) * 128], idb[:],
                               reads=[(kT[hh][1], t4), (bidb, None)], writes=[(bpt, None)], inc=(q4 == 3 and hh == 1))
                    for hh in range(2):
                        H = hp * 2 + hh
                        fw.op(ACT, lambda: nc.scalar.activation(out=kt[hh][0][:, t4 * 4:(t4 + 1) * 4, :], in_=pbv[:, :, hh, :], func=AF.Identity,
                                                                scale=cst[:, C_KD2 + H:C_KD2 + H + 1]),
                              reads=[(bpt, None), (bcst, None)], writes=[(kt[hh][1], t4)])
                wv, bwv = w_get("rv%d_%d" % (b, hp), None)
                w_hint("rg%d_%d" % (b, hp), [(0, w_in_d[:, 5120 + hp * 512:5120 + (hp + 1) * 512], 8, 512)])
                for t in range(NT):
                    pt_, bpt = proj_tm(pbank(ALL_BANKS), wv, bwv, 0, 512, xT, bxT, t // 4, t * 128, 512)
                    copy(alt_eng(), vv[:, t, :, :], pt_[:, :].rearrange("p (h e) -> p h e", h=2), reads=[(bpt, None)], writes=[(bvv, t)])
                wg, bwg = w_get("rg%d_%d" % (b, hp), None)
                if hp == 1:
                    fire_next()
                if hp == 0:
                    c0n = 3072 + 256
                    c1n = 3584 + 256
                    w_hint("rqk%d_%d" % (b, 1), [(0, w_in_d[:, c0n:c0n + 256], 8, 256), (2048, w_in_d[:, c1n:c1n + 256], 8, 256)])
                RB_A = [0, 1, 6, 7]

                def emit_scores(n):
                    cs = slice(n * 128, (n + 1) * 128)
                    for hh in range(2):
                        H = hp * 2 + hh
                        ps_, bps = psb(pbank(RB_A))
                        mm(ps_[:, 0:128], kT[hh][0][:, cs], qT[hh][0][:, cs], start=True, stop=True,
                           reads=[(kT[hh][1], n // 4), (qT[hh][1], n // 4)], writes=[(bps, None)])
                        sm_, bsm_ = stm[(n % 2) * 2 + hh]
                        fw.op(DVE, lambda: nc.vector.tensor_tensor(out=sm_[:], in0=ps_[:, 0:128], in1=cst[:, C_MRET + H * 128:C_MRET + (H + 1) * 128], op=ALU.mult),
                              reads=[(bps, None), (bcst, None)], writes=[(bsm_, None)])

                def emit_z(n):
                    for hh in range(2):
                        po_, bpo = psb(2 + (n % 2) * 2 + hh)
                        fw.op(DVE, lambda: nc.vector.tensor_scalar(out=zt[:, (n // 4) % 2, n % 4, hh, :], in0=po_[:, 0:256], scalar1=mv[:, n % 2, hh, 0:1], scalar2=mv[:, n % 2, hh, 3:4],
                                                                   op0=ALU.subtract, op1=ALU.mult),
                              reads=[(bpo, None), (bmv, (n % 2, hh))], writes=[(bzt, ((n // 4) % 2, n % 4, hh))])

                def emit_group(tg):
                    for hh in range(2):
                        H = hp * 2 + hh
                        for ec in range(2):
                            ch = H * 2 + ec
                            pt_, bpt = psb(pbank(RB_A))
                            pb = pt_[:].bitcast(BF16)
                            for q4 in range(4):
                                tr(pb[:, q4 * 128:(q4 + 1) * 128], zt[:, tg % 2, q4, hh, ec * 128:(ec + 1) * 128], idb[:],
                                   reads=[(bzt, (tg % 2, q4, hh)), (bidb, None)], writes=[(bpt, None)], inc=(q4 == 3))
                            pg_, bpg = proj_fm(pbank(RB_A), wg, bwg, hh * 256 + ec * 128, 128, xT, bxT, tg, tg * 512, 512, 512)
                            th_, bth = thb[ec]
                            sg_, bsg = sgb[ec]
                            yz_, byz = yzb[ec]
                            fw.op(ACT, lambda: nc.scalar.activation(out=th_[:], in_=pg_[:, :], func=AF.Tanh, scale=0.5), reads=[(bpg, None)], writes=[(bth, None)])
                            fw.op(DVE, lambda: nc.vector.scalar_tensor_tensor(out=sg_[:], in0=th_[:], scalar=1.0, in1=pg_[:, :], op0=ALU.add, op1=ALU.mult),
                                  reads=[(bth, None), (bpg, None)], writes=[(bsg, None)])
                            fw.op(ACT, lambda: nc.scalar.activation(out=yz_[:], in_=pb[:, 0:512], func=AF.Identity, scale=pv[:, 24 + ch:25 + ch], bias=pv[:, 32 + ch:33 + ch]),
                                  reads=[(bpt, None), (bpv, None)], writes=[(byz, None)])
                            fw.op(DVE, lambda: nc.vector.tensor_tensor(out=yT[:, ch, tg * 512:(tg + 1) * 512], in0=yz_[:], in1=sg_[:], op=ALU.mult),
                                  reads=[(byz, None), (bsg, None)], writes=[(byT, (ch, tg))])

                emit_scores(0)
                for n in range(NT):
                    cs = slice(n * 128, (n + 1) * 128)
                    if n + 1 < NT:
                        emit_scores(n + 1)
                    for hh in range(2):
                        H = hp * 2 + hh
                        sm_, bsm_ = stm[(n % 2) * 2 + hh]
                        po_, bpo = psb(2 + (n % 2) * 2 + hh)
                        mm(po_[:, 0:256], sm_[:], vv[:, n, hh, :], start=True, stop=(n == 0), reads=[(bsm_, None), (bvv, n)], writes=[(bpo, None)])
                        if n > 0:
                            mm(po_[:, 0:256], qT[hh][0][:, cs], st_b[:, hh, :], start=False, stop=True,
                               reads=[(qT[hh][1], n // 4), (bst_b, hh)], writes=[(bpo, None)])
                    if n < NT - 1:
                        for hh in range(2):
                            H = hp * 2 + hh
                            pk_, bpk = psb(pbank(RB_A))
                            mm(pk_[:, 0:256], kt[hh][0][:, n, :], vv[:, n, hh, :], start=True, stop=True,
                               reads=[(kt[hh][1], n // 4), (bvv, n)], writes=[(bpk, None)])
                            if n == 0:
                                fw.op(DVE, lambda: nc.vector.tensor_copy(st_f[:, hh, :], pk_[:, 0:256]), reads=[(bpk, None)], writes=[(bst_f, hh)])
                            else:
                                fw.op(DVE, lambda: nc.vector.scalar_tensor_tensor(out=st_f[:, hh, :], in0=st_f[:, hh, :], scalar=GAMMA[H] ** 128, in1=pk_[:, 0:256],
                                                                                  op0=ALU.mult, op1=ALU.add),
                                      reads=[(bpk, None), (bst_f, hh)], writes=[(bst_f, hh)])
                            fw.op(ACT, lambda: nc.scalar.copy(st_b[:, hh, :], st_f[:, hh, :]), reads=[(bst_f, hh)], writes=[(bst_b, hh)])
                    for hh in range(2):
                        po_, bpo = psb(2 + (n % 2) * 2 + hh)
                        fw.op(DVE, lambda: nc.vector.bn_stats(out=bns[:, hh, 0:6], in_=po_[:, 0:256]), reads=[(bpo, None)], writes=[(bbns, hh)])
                        fw.op(DVE, lambda: nc.vector.bn_aggr(out=mv[:, n % 2, hh, 0:2], in_=bns[:, hh, 0:6]), reads=[(bbns, hh)], writes=[(bmv, (n % 2, hh))])
                        fw.op(POOL, lambda: nc.gpsimd.tensor_scalar(out=mv[:, n % 2, hh, 2:3], in0=mv[:, n % 2, hh, 1:2], scalar1=EPS, scalar2=None, op0=ALU.add),
                              reads=[(bmv, (n % 2, hh))], writes=[(bmv, (n % 2, hh))])
                        fw.op(POOL, lambda: nc.gpsimd.tensor_tensor(out=mv[:, n % 2, hh, 3:4], in0=mv[:, n % 2, hh, 2:3], in1=nhalf, op=ALU.pow),
                              reads=[(bmv, (n % 2, hh)), (bcst, None)], writes=[(bmv, (n % 2, hh))])
                    if n >= 1:
                        emit_z(n - 1)
                    if n >= 5 and (n - 5) % 4 == 0:
                        emit_group((n - 5) // 4)
                emit_z(NT - 1)
                emit_group(3)

        def stage_mem(b, yT, byT):
            memT, bmemT = carve(arena, RB0 + 0, [128, 8, 256], BF16, "memT")
            mKT, bmKT = carve(arena, RB0 + 4 * KB, [128, 8, 256], BF16, "mKT")
            mV1, bmV1 = carve(arena, RB0 + 8 * KB, [128, 2, 4, 258], BF16, "mV1")
            mq = [carve(arena, RB0 + 13 * KB + i * 8 * KB, [128, 2, S], BF16, "mq%d" % i) for i in range(2)]
            ymts = [carve(tarena, i * 2 * KB, [128, 4, 256], BF16, "ymt%d" % i) for i in range(2)]
            rr, brr = carve(tarena, 4 * KB, [128, 4], F32, "mrr")
            macc, bmacc = carve(tarena, 5 * KB, [128, 4, 258], F32, "macc")
            load_T(lambda t: mem_d[b, t * 128:(t + 1) * 128, :], 2, memT, bmemT, lambda t: None)
            fw.op(DVE, lambda: nc.vector.memset(mV1[:, :, :, 256:257], 1.0), writes=[(bmV1, "ones")])
            jobs = [("mk%d_%d" % (b, i), [(0, w_kv_d[:, i * 512:(i + 1) * 512], 8, 512)]) for i in range(4)]

            def kvjob(i, w):
                wt, bw = w
                if i == 3:
                    w_hint("mq%d_0" % b, [(0, w_in_d[:, 6144:6144 + 256], 8, 256)])
                if i < 2:
                    for cc in range(4):
                        c = i * 4 + cc
                        pt_, bpt = psb(pbank(ALL_BANKS))
                        for k in range(8):
                            mm(pt_[:, 0:256], wt[:, k * 512 + cc * 128:k * 512 + (cc + 1) * 128], memT[:, k, :], start=(k == 0), stop=(k == 7),
                               reads=[(bw, None), (bmemT, None)], writes=[(bpt, None)])
                        copy(alt_eng(), mKT[:, c, :], pt_[:, 0:256], reads=[(bpt, None)], writes=[(bmKT, c)], scale=1.0 / 16.0)
                else:
                    hf = i - 2
                    for mt in range(2):
                        pt_, bpt = psb(pbank(ALL_BANKS))
                        for k in range(8):
                            mm(pt_[:, :], memT[:, k, mt * 128:(mt + 1) * 128], wt[:, k * 512:(k + 1) * 512], start=(k == 0), stop=(k == 7),
                               reads=[(bw, None), (bmemT, None)], writes=[(bpt, None)])
                        copy(alt_eng(), mV1[:, mt, hf * 2:hf * 2 + 2, 0:256], pt_[:, :].rearrange("p (h e) -> p h e", h=2),
                             reads=[(bpt, None)], writes=[(bmV1, (mt, hf))])
            run_jobs(jobs, kvjob, fire=False)
            for H in range(4):
                wq_, bwq = w_get("mq%d_%d" % (b, H), None)
                if H < 3:
                    w_hint("mq%d_%d" % (b, H + 1), [(0, w_in_d[:, 6144 + (H + 1) * 256:6144 + (H + 2) * 256], 8, 256)])
                else:
                    fire_next()
                mqt, bmq = mq[H % 2]
                for dc in range(2):
                    for tg in range(4):
                        pt_, bpt = proj_fm(pbank([6, 7]), wq_, bwq, dc * 128, 128, xT, bxT, tg, tg * 512, 512, 256)
                        copy(alt_eng(), mqt[:, dc, tg * 512:(tg + 1) * 512], pt_[:, :], reads=[(bpt, None)], writes=[(bmq, tg)])
                for tg in range(4):
                    for mt in range(2):
                        pst, bpst = psb(pbank([4, 5]))
                        for dc in range(2):
                            mm(pst[:, :], mKT[:, H * 2 + dc, mt * 128:(mt + 1) * 128], mqt[:, dc, tg * 512:(tg + 1) * 512], start=(dc == 0), stop=(dc == 1),
                               reads=[(bmKT, None), (bmq, tg)], writes=[(bpst, None)])
                        ptt, bptt = PT[(tg * 2 + mt) % 4]
                        fw.op(ACT, lambda: nc.scalar.activation(out=ptt[:], in_=pst[:, :], func=AF.Exp), reads=[(bpst, None)], writes=[(bptt, None)])
                        for ii in range(4):
                            pa, bpa = psb(ii)
                            mm(pa[:, 0:257], ptt[:, ii * 128:(ii + 1) * 128], mV1[:, mt, H, 0:257], start=(mt == 0), stop=(mt == 1),
                               reads=[(bptt, None), (bmV1, None)], writes=[(bpa, None)], inc=True)
                    run_deferred()
                    ymt, bymt = ymts[(H * 4 + tg) % 2]
                    for ii in range(4):
                        pa, bpa = psb(ii)
                        fw.op(DVE, lambda: nc.vector.tensor_copy(macc[:, ii, 0:257], pa[:, 0:257]), reads=[(bpa, None)], writes=[(bmacc, ii)])
                    fw.op(DVE, lambda: nc.vector.reciprocal(rr[:, 0:4], macc[:, :, 256]), reads=[(bmacc, None)], writes=[(brr, None)])
                    fw.op(DVE, lambda: nc.vector.tensor_tensor(out=ymt[:], in0=macc[:, :, 0:256], in1=rr[:, 0:4].unsqueeze(2).broadcast_to([128, 4, 256]), op=ALU.mult),
                          reads=[(bmacc, None), (brr, None)], writes=[(bymt, None)])

                    def part2(H=H, tg=tg, ymt=ymt, bymt=bymt):
                        for ec in range(2):
                            pt_, bpt = psb(pbank([6, 7]))
                            pb = pt_[:].bitcast(BF16)
                            for ii in range(4):
                                tr(pb[:, ii * 128:(ii + 1) * 128], ymt[:, ii, ec * 128:(ec + 1) * 128], idb[:],
                                   reads=[(bymt, ii), (bidb, None)], writes=[(bpt, None)], inc=(ii == 3))
                            copy(alt_eng(), yT[:, H * 2 + ec, tg * 512:(tg + 1) * 512], pb[:, 0:512], reads=[(bpt, None)], writes=[(byT, (H * 2 + ec, tg))])
                    deferred.append(part2)
            run_deferred()

        def fold(b, n, yT, byT, mg, bmg, first):
            thb = [carve(tarena, i * KB, [128, 512], BF16, "fth%d" % i) for i in range(2)]
            tmp = [carve(tarena, 2 * KB + i * 2 * KB, [128, 512], F32, "ftmp%d" % i) for i in range(2)]
            jobs = []
            for dc in range(8):
                g0 = 7168 + n * 1024 + dc * 128
                jobs.append(("fold%d_%d_%d" % (b, n, dc), [(0, w_br_d[n][:, dc * 128:(dc + 1) * 128], 8, 128), (1024, w_in_d[:, g0:g0 + 128], 8, 128)]))

            def job(dc, w):
                wt, bw = w
                for tg in range(4):
                    pbd, bpbd = proj_fm(pbank(ALL_BANKS), wt, bw, 0, 128, yT, byT, None, tg * 512, 512, 128)
                    pg_, bpg = proj_fm(pbank(ALL_BANKS), wt, bw, 1024, 128, xT, bxT, tg, tg * 512, 512, 128)
                    th_, bth = thb[tg % 2]
                    col = n * 8 + dc
                    fw.op(ACT, lambda: nc.scalar.activation(out=th_[:], in_=pg_[:, :], func=AF.Tanh, scale=0.5, bias=pv[:, col:col + 1]),
                          reads=[(bpg, None), (bpv, None)], writes=[(bth, None)])
                    dst = mg[:, dc, tg * 512:(tg + 1) * 512]
                    if first:
                        fw.op(DVE, lambda: nc.vector.scalar_tensor_tensor(out=dst, in0=th_[:], scalar=1.0, in1=pbd[:, :], op0=ALU.add, op1=ALU.mult),
                              reads=[(bth, None), (bpbd, None)], writes=[(bmg, (dc, tg))])
                    else:
                        tm_, btm = tmp[tg % 2]
                        fw.op(DVE, lambda: nc.vector.scalar_tensor_tensor(out=tm_[:], in0=th_[:], scalar=1.0, in1=pbd[:, :], op0=ALU.add, op1=ALU.mult),
                              reads=[(bth, None), (bpbd, None)], writes=[(btm, None)])
                        fw.op(DVE, lambda: nc.vector.tensor_tensor(out=dst, in0=tm_[:], in1=dst, op=ALU.add),
                              reads=[(btm, None), (bmg, (dc, tg))], writes=[(bmg, (dc, tg))])
            run_jobs(jobs, job)

        def layer_norm(h, bh, hkey, bns, bbns, mv, bmv, out, bout, okey):
            hv = h.rearrange("p (a f) -> p a f", a=2)
            for a_ in range(2):
                fw.op(DVE, lambda: nc.vector.bn_stats(out=bns[:, a_, 0:6], in_=hv[:, a_, :]), reads=[(bh, hkey)], writes=[(bbns, a_)])
            fw.op(DVE, lambda: nc.vector.bn_aggr(out=mv[:, 0:2], in_=bns[:, :, 0:6]), reads=[(bbns, None)], writes=[(bmv, None)])
            fw.op(POOL, lambda: nc.gpsimd.tensor_scalar(out=mv[:, 2:3], in0=mv[:, 1:2], scalar1=EPS, scalar2=None, op0=ALU.add), reads=[(bmv, None)], writes=[(bmv, None)])
            fw.op(POOL, lambda: nc.gpsimd.tensor_tensor(out=mv[:, 3:4], in0=mv[:, 2:3], in1=nhalf, op=ALU.pow), reads=[(bmv, None), (bcst, None)], writes=[(bmv, None)])
            fw.op(DVE, lambda: nc.vector.tensor_scalar(out=out, in0=h, scalar1=mv[:, 0:1], scalar2=mv[:, 3:4], op0=ALU.subtract, op1=ALU.mult),
                  reads=[(bh, hkey), (bmv, None)], writes=[(bout, okey)])
            fw.op(DVE, lambda: nc.vector.tensor_tensor(out=out, in0=out, in1=lnb[:, 0, :], op=ALU.mult), reads=[(bout, okey), (blnb, None)], writes=[(bout, okey)])
            fw.op(DVE, lambda: nc.vector.tensor_tensor(out=out, in0=out, in1=lnb[:, 1, :], op=ALU.add), reads=[(bout, okey), (blnb, None)], writes=[(bout, okey)])

        def stage5(b, mg, bmg):
            acc, bacc = carve(arena, 0, [128, NT, D], F32, "acc")
            xr = [carve(tarena, i * 4 * KB, [128, D], F32, "xr%d" % i) for i in range(2)]
            hb = [carve(tarena, 8 * KB + i * 4 * KB, [128, D], F32, "hb%d" % i) for i in range(2)]
            xr.append(carve(arena, 64 * KB, [128, D], F32, "xr2"))
            hb.append(carve(arena, 68 * KB, [128, D], F32, "hb2"))
            xrc = [fw.counter("xrc%d_%d" % (b, i), 16) for i in range(3)]
            bnsl = [carve(tarena, 16 * KB + i * 64, [128, 2, 8], F32, "bns5_%d" % i) for i in range(2)]
            mvl = [carve(tarena, 16 * KB + 128 + i * 32, [128, 8], F32, "mv5_%d" % i) for i in range(2)]
            rt, brt = carve(tarena, 16 * KB + 192, [128, 600], F32, "rt")
            tlTl = [(xst[i][0][:].rearrange("p (c t) -> p c t", c=8), xst[i][1]) for i in range(2)]
            fw.dma(SP, lnb[:, 0, :], bc_d[:, 0:D].partition_broadcast(128), writes=[(blnb, None)], counter=blnb.counter)
            fw.dma(SP, lnb[:, 1, :], bc_d[:, D:2 * D].partition_broadcast(128), writes=[(blnb, None)], counter=blnb.counter)
            wo = []
            wo.append(w_get("wo%d_0" % b, [(0, w_o_d[:, 0:512], 8, 512)]))
            wo.append(w_get("wo%d_1" % b, [(0, w_o_d[:, 512:1024], 8, 512)]))
            prs = {}
            S5_BANKS = [0, 1, 2, 3, 4, 5]

            def phA(t):
                xr_, bxr = xr[t % 3]
                hb_, bhb = hb[t % 3]
                fw.dma(SP, xr_, x_d[b, t * 128:(t + 1) * 128, :], writes=[(bxr, None)], counter=xrc[t % 3])
                fw.op(ACT, lambda: nc.scalar.mul(xr_, xr_, ALPHA), reads=[(bxr, None)], writes=[(bxr, None)])
                for hf in range(2):
                    wt, bw = wo[hf]
                    pt_, bpt = proj_tm(pbank(S5_BANKS), wt, bw, 0, 512, mg, bmg, None, t * 128, 512)
                    fw.op(DVE, lambda: nc.vector.scalar_tensor_tensor(out=hb_[:, hf * 512:(hf + 1) * 512], in0=pt_[:, :], scalar=0.5, in1=xr_[:, hf * 512:(hf + 1) * 512],
                                                                      op0=ALU.mult, op1=ALU.add),
                          reads=[(bpt, None), (bxr, None)], writes=[(bhb, hf)])

            def phB1(t):
                hb_, bhb = hb[t % 3]
                bns, bbns = bnsl[t % 2]
                mv, bmv = mvl[t % 2]
                hv = hb_.rearrange("p (a f) -> p a f", a=2)
                for a_ in range(2):
                    fw.op(DVE, lambda: nc.vector.bn_stats(out=bns[:, a_, 0:6], in_=hv[:, a_, :]), reads=[(bhb, None)], writes=[(bbns, a_)])
                fw.op(DVE, lambda: nc.vector.bn_aggr(out=mv[:, 0:2], in_=bns[:, :, 0:6]), reads=[(bbns, None)], writes=[(bmv, None)])
                fw.op(POOL, lambda: nc.gpsimd.tensor_scalar(out=mv[:, 2:3], in0=mv[:, 1:2], scalar1=EPS, scalar2=None, op0=ALU.add), reads=[(bmv, None)], writes=[(bmv, None)])
                fw.op(POOL, lambda: nc.gpsimd.tensor_tensor(out=mv[:, 3:4], in0=mv[:, 2:3], in1=nhalf, op=ALU.pow), reads=[(bmv, None), (bcst, None)], writes=[(bmv, None)])
                fw.op(POOL, lambda: nc.gpsimd.tensor_tensor(out=mv[:, 4:5], in0=mv[:, 0:1], in1=mv[:, 3:4], op=ALU.mult), reads=[(bmv, None)], writes=[(bmv, None)])
                fw.op(POOL, lambda: nc.gpsimd.tensor_scalar(out=mv[:, 5:6], in0=mv[:, 4:5], scalar1=-1.0, scalar2=None, op0=ALU.mult), reads=[(bmv, None)], writes=[(bmv, None)])

            def phB2(t):
                xr_, bxr = xr[t % 3]
                hb_, bhb = hb[t % 3]
                mv, bmv = mvl[t % 2]
                fw.op(ACT, lambda: nc.scalar.activation(out=hb_, in_=hb_, func=AF.Identity, scale=mv[:, 3:4], bias=mv[:, 5:6]),
                      reads=[(bhb, None), (bmv, None)], writes=[(bhb, None)])
                fw.op(DVE, lambda: nc.vector.tensor_tensor(out=hb_, in0=hb_, in1=lnb[:, 0, :], op=ALU.mult), reads=[(bhb, None), (blnb, None)], writes=[(bhb, None)])
                fw.op(POOL, lambda: nc.gpsimd.tensor_tensor(out=hb_, in0=hb_, in1=lnb[:, 1, :], op=ALU.add), reads=[(bhb, None), (blnb, None)], writes=[(bhb, None)])
                tb = xr_.bitcast(BF16)
                thi = tb[:, 0:D]
                tlo = tb[:, D:2 * D]
                fw.op(ACT, lambda: nc.scalar.copy(thi, hb_), reads=[(bhb, None)], writes=[(bxr, None)])
                fw.op(DVE, lambda: nc.vector.tensor_tensor(out=tlo, in0=hb_, in1=thi, op=ALU.subtract), reads=[(bhb, None), (bxr, None)], writes=[(bxr, None)])

            def phC(t):
                xr_, bxr = xr[t % 3]
                hb_, bhb = hb[t % 3]
                tlT, btlT = tlTl[t % 2]
                tb = xr_.bitcast(BF16)
                thi = tb[:, 0:D]
                tlo = tb[:, D:2 * D]
                pth, bpth = psb(pbank(S5_BANKS))
                ptl, bptl = psb(pbank(S5_BANKS))
                pthb = pth[:].bitcast(BF16)
                ptlb = ptl[:].bitcast(BF16)
                for c in range(8):
                    tr(pthb[:, c * 128:(c + 1) * 128], thi[:, c * 128:(c + 1) * 128], idb[:], reads=[(bxr, None), (bidb, None)], writes=[(bpth, None)], inc=(c == 7))
                for c in range(8):
                    tr(ptlb[:, c * 128:(c + 1) * 128], tlo[:, c * 128:(c + 1) * 128], idb[:], reads=[(bxr, None), (bidb, None)], writes=[(bptl, None)], inc=(c == 7))
                fw.op(ACT, lambda: nc.scalar.copy(xT[:, :, t * 128:(t + 1) * 128], pthb.rearrange("p (c t) -> p c t", c=8)),
                      reads=[(bpth, None)], writes=[(bxT, t // 4)])
                fw.op(DVE, lambda: nc.vector.tensor_copy(tlT, ptlb.rearrange("p (c t) -> p c t", c=8)), reads=[(bptl, None)], writes=[(btlT, None)])
                if t % 4 == 0:
                    prs[t // 4] = psb(6 + (t // 4) % 2)
                pr_, bpr = prs[t // 4]
                q4 = t % 4
                nmm = 0
                for k in range(8):
                    for (lt, blt, lkey, rt_, brt_) in ((xT[:, k, t * 128:(t + 1) * 128], bxT, t // 4, wrh, bwrh),
                                                       (tlT[:, k, :], btlT, None, wrh, bwrh),
                                                       (xT[:, k, t * 128:(t + 1) * 128], bxT, t // 4, wrl, bwrl)):
                        mm(pr_[:, q4 * 20:(q4 + 1) * 20], lt, rt_[:, k * 20:(k + 1) * 20], start=(nmm == 0 and q4 == 0), stop=(nmm == 23),
                           reads=[(blt, lkey), (brt_, None)], writes=[(bpr, q4)], inc=(nmm == 23), sgc=True)
                        nmm += 1
                fw.op(ACT, lambda: nc.scalar.mul(acc[:, t, :], hb_, ALPHA), reads=[(bhb, None)], writes=[(bacc, t)])

            def phD(g):
                pr_, bpr = prs[g]
                R = [(brt, None)]
                o = [0]

                def al(n):
                    a_ = o[0]
                    o[0] += n
                    return rt[:, a_:a_ + n]
                lg = al(80).rearrange("p (t c) -> p t c", t=4)
                fw.op(DVE, lambda: nc.vector.tensor_tensor(out=lg, in0=pr_[:, 0:80].rearrange("p (t c) -> p t c", t=4), in1=brb[:].unsqueeze(1).broadcast_to([128, 4, 20]), op=ALU.add),
                      reads=[(bpr, None), (bbrb, None)], writes=R)
                gmax = al(4)
                fw.op(DVE, lambda: nc.vector.reduce_max(out=gmax, in_=lg[:, :, 0:4], axis=AX.X), reads=R, writes=R)
                dg = al(16).rearrange("p (t c) -> p t c", t=4)
                fw.op(DVE, lambda: nc.vector.tensor_tensor(out=dg, in0=lg[:, :, 0:4], in1=gmax.unsqueeze(2).broadcast_to([128, 4, 4]), op=ALU.subtract), reads=R, writes=R)
                eg = al(16).rearrange("p (t c) -> p t c", t=4)
                fw.op(ACT, lambda: nc.scalar.activation(out=eg, in_=dg, func=AF.Exp), reads=R, writes=R)
                sumg = al(4)
                fw.op(DVE, lambda: nc.vector.reduce_sum(out=sumg, in_=eg, axis=AX.X), reads=R, writes=R)
                ptop = al(4)
                fw.op(DVE, lambda: nc.vector.reciprocal(ptop, sumg), reads=R, writes=R)
                pen = al(16).rearrange("p (t c) -> p t c", t=4)
                fw.op(DVE, lambda: nc.vector.tensor_scalar(out=pen, in0=dg, scalar1=0.0, scalar2=None, op0=ALU.is_equal), reads=R, writes=R)
                fw.op(DVE, lambda: nc.vector.tensor_scalar(out=pen, in0=pen, scalar1=-1.0, scalar2=1e30, op0=ALU.add, op1=ALU.mult), reads=R, writes=R)
                elm = al(64).rearrange("p (t c) -> p t c", t=4)
                fw.op(DVE, lambda: nc.vector.tensor_tensor(out=elm.rearrange("p t (g e) -> p t g e", g=4), in0=lg[:, :, 4:20].rearrange("p t (g e) -> p t g e", g=4),
                                                           in1=pen.unsqueeze(3).broadcast_to([128, 4, 4, 4]), op=ALU.add), reads=R, writes=R)
                top8 = al(32).rearrange("p (t c) -> p t c", t=4)
                for q4 in range(4):
                    fw.op(DVE, lambda: nc.vector.max(out=top8[:, q4, :], in_=elm[:, q4, :]), reads=R, writes=R)
                d21 = al(4)
                fw.op(DVE, lambda: nc.vector.tensor_tensor(out=d21, in0=top8[:, :, 1], in1=top8[:, :, 0], op=ALU.subtract), reads=R, writes=R)
                e2 = al(4)
                fw.op(ACT, lambda: nc.scalar.activation(out=e2, in_=d21, func=AF.Exp), reads=R, writes=R)
                w1 = al(4)
                fw.op(DVE, lambda: nc.vector.tensor_scalar(out=w1, in0=e2, scalar1=1.0, scalar2=None, op0=ALU.add), reads=R, writes=R)
                fw.op(DVE, lambda: nc.vector.reciprocal(w1, w1), reads=R, writes=R)
                w1p = al(4)
                fw.op(DVE, lambda: nc.vector.tensor_tensor(out=w1p, in0=w1, in1=ptop, op=ALU.mult), reads=R, writes=R)
                w2p = al(4)
                fw.op(DVE, lambda: nc.vector.tensor_tensor(out=w2p, in0=w1p, in1=e2, op=ALU.mult), reads=R, writes=R)
                c1 = al(64).rearrange("p (t c) -> p t c", t=4)
                c2 = al(64).rearrange("p (t c) -> p t c", t=4)
                fw.op(DVE, lambda: nc.vector.tensor_tensor(out=c1, in0=elm, in1=top8[:, :, 0:1].broadcast_to([128, 4, 16]), op=ALU.is_equal), reads=R, writes=R)
                fw.op(DVE, lambda: nc.vector.tensor_tensor(out=c1, in0=c1, in1=w1p.unsqueeze(2).broadcast_to([128, 4, 16]), op=ALU.mult), reads=R, writes=R)
                fw.op(DVE, lambda: nc.vector.tensor_tensor(out=c2, in0=elm, in1=top8[:, :, 1:2].broadcast_to([128, 4, 16]), op=ALU.is_equal), reads=R, writes=R)
                fw.op(DVE, lambda: nc.vector.tensor_tensor(out=c2, in0=c2, in1=w2p.unsqueeze(2).broadcast_to([128, 4, 16]), op=ALU.mult), reads=R, writes=R)
                fw.op(DVE, lambda: nc.vector.tensor_tensor(out=comb[:, g * 4:(g + 1) * 4, :], in0=c1, in1=c2, op=ALU.add), reads=R, writes=[(bcomb, g)])

            for step in range(NT + 3):
                if step >= 3:
                    phC(step - 3)
                    if (step - 3) % 4 == 3 and S5LVL >= 5:
                        phD((step - 3) // 4)
                if 2 <= step <= NT + 1:
                    phB2(step - 2)
                if 1 <= step <= NT:
                    phB1(step - 1)
                if step < NT:
                    phA(step)
            return acc, bacc

        def moe(b, acc, bacc):
            ln2_tile = ln2_setup(b, acc, bacc)
            base = 64 * KB
            hT = [carve(arena, base + i * 4 * KB, [128, 4, 512], BF16, "hT%d" % i) for i in range(2)]
            sab = [carve(arena, base + 8 * KB + i * KB, [128, 512], BF16, "sa%d" % i) for i in range(2)]
            extra = []
            for i in range(3):
                ap_, b_ = carve(arena, base + 10 * KB + i * 8 * KB, [128, 4096], BF16, "wx%d" % i)
                b_.counter = xslot_counters[i]
                extra.append((ap_, b_))
            assert not wq["fifo"]
            saved = (wq["slots"], wq["next"])
            wq["slots"] = list(wslots) + extra
            wq["next"] = 0

            def jobs_for(e):
                return [("w1_%d_%d" % (b, e), [(0, w1_d[e], 8, 512)]), ("w3_%d_%d" % (b, e), [(0, w3_d[e], 8, 512)]), ("w2_%d_%d" % (b, e), [(0, w2_d[e], 4, 1024)])]

            for j in jobs_for(0):
                w_hint(*j)
            pending = []
            for e in range(16):
                ws = [w_get(*j) for j in jobs_for(e)]
                (w1t, bw1), (w3t, bw3), (w2t, bw2) = ws
                for tg in range(4):
                    hT_, bhT = hT[tg % 2]
                    for f in range(4):
                        pa, bpa = proj_fm(pbank(ALL_BANKS), w1t, bw1, f * 128, 128, xT, bxT, tg, tg * 512, 512, 512)
                        pb_, bpb = proj_fm(pbank(ALL_BANKS), w3t, bw3, f * 128, 128, xT, bxT, tg, tg * 512, 512, 512)
                        sa_, bsa = sab[f % 2]
                        fw.op(ACT, lambda: nc.scalar.activation(out=sa_[:], in_=pa[:, :], func=AF.Silu), reads=[(bpa, None)], writes=[(bsa, None)])
                        fw.op(DVE, lambda: nc.vector.tensor_tensor(out=hT_[:, f, :], in0=sa_[:], in1=pb_[:, :], op=ALU.mult),
                              reads=[(bsa, None), (bpb, None)], writes=[(bhT, f)])
                    while pending:
                        pending.pop(0)()
                    if tg == 0 and e + 1 < 16:
                        for j in jobs_for(e + 1):
                            w_hint(*j)

                    def outp(e=e, tg=tg, hT_=hT_, bhT=bhT, w2t=w2t, bw2=bw2):
                        for tt in range(4):
                            t = tg * 4 + tt
                            for hf in range(2):
                                po, bpo = psb(pbank(ALL_BANKS))
                                for f in range(4):
                                    mm(po[:, :], hT_[:, f, tt * 128:(tt + 1) * 128], w2t[:, f * 1024 + hf * 512:f * 1024 + (hf + 1) * 512],
                                       start=(f == 0), stop=(f == 3), reads=[(bhT, None), (bw2, None)], writes=[(bpo, None)])
                                dst = acc[:, t, hf * 512:(hf + 1) * 512]
                                fw.op(DVE, lambda: nc.vector.scalar_tensor_tensor(out=dst, in0=po[:, :], scalar=comb[:, t, e:e + 1], in1=dst, op0=ALU.mult, op1=ALU.add),
                                      reads=[(bpo, None), (bcomb, t // 4), (bacc, t)], writes=[(bacc, t)])
                            if e == 15:
                                ln2_tile(t)
                    pending.append(outp)
            while pending:
                pending.pop(0)()
            assert not wq["fifo"]
            wq["slots"], wq["next"] = saved

        def ln2_setup(b, acc, bacc):
            ot = [carve(tarena, i * 4 * KB, [128, D], F32, "ot%d" % i) for i in range(2)]
            bns, bbns = carve(tarena, 16 * KB, [128, 2, 8], F32, "bns6")
            mv, bmv = carve(tarena, 16 * KB + 64, [128, 4], F32, "mv6")
            fw.dma(SP, lnb[:, 0, :], bc_d[:, 2 * D:3 * D].partition_broadcast(128), writes=[(blnb, None)], counter=blnb.counter)
            fw.dma(SP, lnb[:, 1, :], bc_d[:, 3 * D:4 * D].partition_broadcast(128), writes=[(blnb, None)], counter=blnb.counter)

            def ln2_tile(t):
                ot_, bot = ot[t % 2]
                layer_norm(acc[:, t, :], bacc, t, bns, bbns, mv, bmv, ot_, bot, None)
                fw.dma(SP, out_d[b, t * 128:(t + 1) * 128, :], ot_, reads=[(bot, None)], counter=out_counters[t % 2])
            return ln2_tile

        augcs = [fw.counter("augc%d" % i, 16) for i in range(4)]
        xslot_counters = [fw.counter("xslotc%d" % i, 16) for i in range(3)]

        dumps = []
        for b in range(nb):
            load_T(lambda t: x_d[b, t * 128:(t + 1) * 128, :], NT, xT, bxT, lambda t: t // 4)
            yT, byT = carve(arena, RA1, [128, 8, S], BF16, "yT")
            mg, bmg = carve(arena, RA2, [128, 8, S], BF16, "mg")
            if stop_after == "s5only":
                fw.op(DVE, lambda: nc.vector.memset(mg[:], 0.25), writes=[(bmg, None)])
                acc, bacc = stage5(b, mg, bmg)
                break
            def fold_first(n):
                g0 = 7168 + n * 1024
                return [("fold%d_%d_%d" % (b, n, 0), [(0, w_br_d[n][:, 0:128], 8, 128), (1024, w_in_d[:, g0:g0 + 128], 8, 128)])]
            nxt[0] = fold_first(0) if stop_after != "da" else None
            stage_da(b, yT, byT)
            if stop_after == "da" and b == nb - 1:
                dumps.append((yT, byT))
                break
            nxt[0] = [("rqk%d_%d" % (b, 0), [(0, w_in_d[:, 3072:3072 + 256], 8, 256), (2048, w_in_d[:, 3584:3584 + 256], 8, 256)])]
            fold(b, 0, yT, byT, mg, bmg, True)
            yT, byT = carve(arena, RA1, [128, 8, S], BF16, "yT")
            nxt[0] = fold_first(1) if stop_after != "ret" else None
            stage_ret(b, yT, byT)
            if stop_after == "ret" and b == nb - 1:
                dumps.append((yT, byT))
                break
            nxt[0] = [("mk%d_%d" % (b, 0), [(0, w_kv_d[:, 0:512], 8, 512)])]
            fold(b, 1, yT, byT, mg, bmg, False)
            yT, byT = carve(arena, RA1, [128, 8, S], BF16, "yT")
            nxt[0] = fold_first(2) if stop_after != "mem" else None
            stage_mem(b, yT, byT)
            if stop_after == "mem" and b == nb - 1:
                dumps.append((yT, byT))
                break
            nxt[0] = [("wo%d_0" % b, [(0, w_o_d[:, 0:512], 8, 512)]), ("wo%d_1" % b, [(0, w_o_d[:, 512:1024], 8, 512)])] if stop_after != "fold" else None
            fold(b, 2, yT, byT, mg, bmg, False)
            if stop_after == "fold" and b == nb - 1:
                dumps.append((mg, bmg))
                break
            acc, bacc = stage5(b, mg, bmg)
            if stop_after == "s5" and b == nb - 1:
                break
            moe(b, acc, bacc)
        run_deferred()
        if dbg and dumps:
            t_, b_ = dumps[0]
            dc_ = fw.counter("dbgc", 16)
            for c in range(8):
                fw.dma(POOL, dbg_d[:, c * S:(c + 1) * S], t_[:, c, :], reads=[(b_, None)], counter=dc_)
            SP.eng.wait_ge(dc_.sem, dc_.n)
        if dbg and stop_after in ("s5", "s5only"):
            dc_ = fw.counter("dbgc", 16)
            for t in range(NT):
                fw.dma(SP, out_d[0, t * 128:(t + 1) * 128, :], acc[:, t, :], reads=[(bacc, None)], counter=dc_)
            fw.dma(SP, dbg_d[:, 0:256], comb[:].rearrange("p a b -> p (a b)"), reads=[(bcomb, None)], counter=dc_)
            SP.eng.wait_ge(dc_.sem, dc_.n)
        for c in out_counters:
            if c.n > 0:
                SP.eng.wait_ge(c.sem, c.n)
        stats = {e.name: (e.nins, e.nwait) for e in (PE, ACT, DVE, POOL, SP)}
        print("kernel build: sems", fw.nsem, "ins/waits", stats)
    return nc


def _consts():
    cst = np.zeros((128, C_N), np.float32)
    cst[:, C_ID:C_ID + 128] = np.eye(128, dtype=np.float32)
    kl = np.arange(128)[:, None]
    ql = np.arange(128)[None, :]
    cst[:, C_MASK:C_MASK + 128] = np.where(kl > ql, MASKNEG, 0.0)
    for h in range(4):
        g = GAMMA[h]
        cst[:, C_MRET + h * 128:C_MRET + (h + 1) * 128] = np.where(ql >= kl, np.power(g, -(kl + 1.0)), 0.0)
        cst[:, C_DQ + h * 128:C_DQ + (h + 1) * 128] = np.power(g, ql + 1.0) * np.ones((128, 1))
        cst[:, C_KDEC + h] = np.power(g, 127.0 - np.arange(128)) * (128.0 ** -0.5)
        cst[:, C_KD2 + h] = np.power(g, 127.0 - np.arange(128))
    cst[:, C_NH] = -0.5
    aug = np.zeros((2, 4, S), np.float32)
    t = np.arange(S)
    aug[0, 0] = -(t // 128) * 128.0
    aug[0, 1] = -(t % 128)
    aug[0, 2] = 1.0
    aug[0, 3] = 1.0
    aug[1, 0] = 1.0
    aug[1, 1] = 1.0
    aug[1, 2] = (t // 128) * 128.0
    aug[1, 3] = (t % 128)
    return cst, aug


_CACHE = {}


def _host_inputs(inputs, nb, cores):
    f = lambda a: np.ascontiguousarray(np.asarray(a, dtype=np.float32))
    cst, aug = _consts()
    pv = np.zeros((128, 41), np.float32)
    pv[:, 0:24] = f(inputs["b_gate"])[0].reshape(24, 128).T
    pv[:, 24:32] = f(inputs["ret_gn_g"])[0].reshape(8, 128).T
    pv[:, 32:40] = f(inputs["ret_gn_b"])[0].reshape(8, 128).T
    pv[:, 40] = f(inputs["da_norm_g"])[0]
    wrc = np.concatenate([f(inputs["w_rg"])[0], f(inputs["w_re"])[0]], axis=1)
    wr = np.ascontiguousarray(wrc.reshape(8, 128, 20).transpose(1, 0, 2).reshape(128, 160))
    bc = np.concatenate([f(inputs["ln1_g"])[0], f(inputs["ln1_b"])[0], f(inputs["ln2_g"])[0], f(inputs["ln2_b"])[0],
                         f(inputs["b_rg"])[0], f(inputs["b_re"])[0], f(inputs["da_lambda"])[0].reshape(-1)])[None, :]
    shared = {
        "w_in": f(inputs["w_in"])[0], "w_mem_kv": f(inputs["w_mem_kv"])[0], "w_branch": f(inputs["w_branch"])[0],
        "w_o": f(inputs["w_o"])[0], "w1": f(inputs["w1"])[0], "w3": f(inputs["w3"])[0], "w2": f(inputs["w2"])[0],
        "cst": cst, "aug": aug, "pv": pv, "wr": wr, "bc": np.ascontiguousarray(bc),
    }
    x = f(inputs["x"])
    mem = f(inputs["mem"])
    maps = []
    for c in cores:
        m = dict(shared)
        m["x"] = np.ascontiguousarray(x[c * nb:(c + 1) * nb])
        m["mem"] = np.ascontiguousarray(mem[c * nb:(c + 1) * nb])
        maps.append(m)
    return maps


def kernel(**inputs):
    nb = 2
    ncores = 8
    if "nc" not in _CACHE:
        _CACHE["nc"] = build(nb=nb)
    nc = _CACHE["nc"]
    maps = _host_inputs(inputs, nb, list(range(ncores)))
    res = run_bass_kernel_spmd(nc, maps, core_ids=list(range(ncores)))
    out = np.concatenate([r["out"] for r in res.results], axis=0)
    return out.astype(np.float32)
```
